# Optimizing a Trainium2 kernel written in Bass

```python
import jax, jax.numpy as jnp
from jax import lax
import numpy as np

D_MODEL = 1024
BATCH = 16
SEQ = 4096
DEPTH = 2

D_MIX = 2 * D_MODEL
D_ATT = D_MIX // 2
D_MLSTM = D_MIX - D_ATT
ATT_HEADS = 8
ATT_HEAD_DIM = D_ATT // ATT_HEADS
DILATED_PATTERNS = ((128, 1), (512, 4), (2048, 16))
MLSTM_HEADS = 4
MLSTM_HEAD_DIM = D_MLSTM // MLSTM_HEADS
QKV_BLOCK = 4
CONV_WIDTH = 4
MLSTM_CHUNK = 64
NORM_EPS = 1e-6
D_IN = 4 * D_ATT + 3 * D_MLSTM

kernel_name = "hybrid_dilated_attn_mlstm_block"


def rmsnorm(x, g):
    xf = x.astype(jnp.float32)
    y = xf * lax.rsqrt(jnp.mean(xf * xf, axis=-1, keepdims=True) + NORM_EPS)
    return (y * g.astype(jnp.float32)).astype(x.dtype)


def alibi_slopes(n_heads):
    h = jnp.arange(1, n_heads + 1, dtype=jnp.float32)
    return jnp.exp2(-8.0 * h / n_heads)


def dilated_window_attention(q, k, v, window, dilation, slopes):
    B, S, H, Dh = q.shape
    span = window // dilation
    blk = span
    lr = S // dilation
    nb = -(-lr // blk)
    lp = nb * blk

    def split(t):
        t = t.reshape(B, lr, dilation, H, Dh)
        t = jnp.pad(t, ((0, 0), (0, lp - lr), (0, 0), (0, 0), (0, 0)))
        return t.reshape(B, nb, blk, dilation, H, Dh)

    def band(t):
        prev = jnp.pad(t[:, :-1], ((0, 0), (1, 0), (0, 0), (0, 0), (0, 0), (0, 0)))
        return jnp.concatenate([prev, t], axis=2)

    qb = split(q)
    kw = band(split(k))
    vw = band(split(v))
    s = jnp.einsum("bnqrhc,bnkrhc->bnrhqk", qb, kw,
                   preferred_element_type=jnp.float32) * (Dh ** -0.5)
    iq = jnp.arange(blk)[:, None]
    ik = jnp.arange(2 * blk)[None, :]
    rel = blk + iq - ik
    key_idx = (jnp.arange(nb) * blk - blk)[:, None, None] + ik[None]
    valid = (rel >= 0) & (rel <= span) & (key_idx >= 0)
    bias = -slopes[:, None, None] * (rel * dilation).astype(jnp.float32)
    s = jnp.where(valid[None, :, None, None], s + bias, -jnp.inf)
    m = jnp.max(s, axis=-1, keepdims=True)
    p = jnp.exp(s - m)
    den = jnp.sum(p, axis=-1)
    o = jnp.einsum("bnrhqk,bnkrhc->bnqrhc", p, vw.astype(jnp.float32))
    den_q = den.transpose(0, 1, 4, 2, 3)
    o = (o / den_q[..., None]).reshape(B, lp * dilation, H, Dh)[:, :S]
    lse = (m[..., 0].transpose(0, 1, 4, 2, 3) + jnp.log(den_q)).reshape(B, lp * dilation, H)[:, :S]
    return o, lse


def causal_depthwise_conv(x, w, b):
    y = lax.conv_general_dilated(
        x, w[:, None, :].astype(x.dtype), window_strides=(1,),
        padding=[(CONV_WIDTH - 1, 0)], dimension_numbers=("NWC", "WIO", "NWC"),
        feature_group_count=x.shape[-1])
    return y + b.astype(x.dtype)


def headwise_linear(t, w):
    B, S, D = t.shape
    tb = t.reshape(B, S, D // QKV_BLOCK, QKV_BLOCK)
    return jnp.einsum("bsnc,ncd->bsnd", tb, w.astype(t.dtype)).reshape(B, S, D)


def mlstm_chunkwise(q, k, v, log_i, log_f):
    B, S, H, D = q.shape
    L = MLSTM_CHUNK
    nc = S // L

    def chunks(t):
        return t.reshape(B, nc, L, H, D).transpose(1, 0, 3, 2, 4)

    def gchunks(t):
        return t.reshape(B, nc, L, H).transpose(1, 0, 3, 2)

    qc = chunks(q)
    kc = chunks(k * (D ** -0.5))
    vc = chunks(v)
    lic = gchunks(log_i)
    gc = jnp.cumsum(gchunks(log_f), axis=-1)
    causal = jnp.arange(L)[:, None] >= jnp.arange(L)[None, :]

    def step(carry, inp):
        C, n, m = carry
        qt, kt, vt, li, g = inp
        dmat = jnp.where(causal, g[..., :, None] - g[..., None, :] + li[..., None, :], -jnp.inf)
        a = g + m[..., None]
        m_t = jnp.maximum(a, jnp.max(dmat, axis=-1))
        p = jnp.exp(dmat - m_t[..., None]) * jnp.einsum("bhtd,bhsd->bhts", qt, kt)
        inter = jnp.exp(a - m_t)
        num = inter[..., None] * jnp.einsum("bhtd,bhde->bhte", qt, C) + jnp.einsum("bhts,bhse->bhte", p, vt)
        den = inter * jnp.einsum("bhtd,bhd->bht", qt, n) + jnp.sum(p, axis=-1)
        h = num / jnp.maximum(jnp.abs(den), jnp.exp(-m_t))[..., None]
        g_end = g[..., -1]
        w_s = g_end[..., None] - g + li
        m_new = jnp.maximum(g_end + m, jnp.max(w_s, axis=-1))
        decay = jnp.exp(g_end + m - m_new)
        ws = jnp.exp(w_s - m_new[..., None])
        C_new = decay[..., None, None] * C + jnp.einsum("bhsd,bhse->bhde", kt * ws[..., None], vt)
        n_new = decay[..., None] * n + jnp.einsum("bhs,bhsd->bhd", ws, kt)
        return (C_new, n_new, m_new), h

    init = (jnp.zeros((B, H, D, D), jnp.float32),
            jnp.zeros((B, H, D), jnp.float32),
            jnp.zeros((B, H), jnp.float32))
    _, hs = lax.scan(step, init, (qc, kc, vc, lic, gc))
    return hs.transpose(1, 0, 3, 2, 4).reshape(B, S, H, D)


def hybrid_layer(x, norm_g, w_in, conv_w, conv_b, w_qm, w_km, w_vm, w_if, b_i, b_f, hn_g, w_out):
    B, S, _ = x.shape
    f32 = jnp.float32
    h = rmsnorm(x, norm_g)
    u = jnp.einsum("bsd,de->bse", h, w_in.astype(h.dtype))
    cuts = [int(c) for c in np.cumsum([D_ATT] * 4 + [D_MLSTM] * 2)]
    q_a, k_a, v_a, z_a, x_m, o_m, z_m = jnp.split(u, cuts, axis=-1)

    heads = lambda t: t.reshape(B, S, ATT_HEADS, ATT_HEAD_DIM)
    slopes = alibi_slopes(ATT_HEADS)
    outs, lses = [], []
    for window, dilation in DILATED_PATTERNS:
        o, l = dilated_window_attention(heads(q_a), heads(k_a), heads(v_a), window, dilation, slopes)
        outs.append(o)
        lses.append(l)
    wts = jax.nn.softmax(jnp.stack(lses), axis=0)
    att = jnp.sum(wts[..., None] * jnp.stack(outs), axis=0).reshape(B, S, D_ATT)
    att = (att * jax.nn.silu(z_a.astype(f32))).astype(x.dtype)

    xc = jax.nn.silu(causal_depthwise_conv(x_m, conv_w, conv_b))
    q_m = headwise_linear(xc, w_qm)
    k_m = headwise_linear(xc, w_km)
    v_m = headwise_linear(x_m, w_vm)
    w_if = w_if.astype(h.dtype)
    gates = (jnp.einsum("bse,eg->bsg", q_m, w_if[:D_MLSTM])
             + jnp.einsum("bse,eg->bsg", k_m, w_if[D_MLSTM:2 * D_MLSTM])
             + jnp.einsum("bse,eg->bsg", v_m, w_if[2 * D_MLSTM:])).astype(f32)
    log_i = gates[..., :MLSTM_HEADS] + b_i.astype(f32)
    log_f = jax.nn.log_sigmoid(gates[..., MLSTM_HEADS:] + b_f.astype(f32))
    mh = lambda t: t.astype(f32).reshape(B, S, MLSTM_HEADS, MLSTM_HEAD_DIM)
    hm = mlstm_chunkwise(mh(q_m), mh(k_m), mh(v_m), log_i, log_f)
    hm = jax.nn.sigmoid(mh(o_m)) * hm
    mu = jnp.mean(hm, axis=-1, keepdims=True)
    var = jnp.mean(jnp.square(hm - mu), axis=-1, keepdims=True)
    hm = (hm - mu) * lax.rsqrt(var + NORM_EPS) * hn_g.astype(f32).reshape(MLSTM_HEADS, MLSTM_HEAD_DIM)
    mls = (hm.reshape(B, S, D_MLSTM) * jax.nn.silu(z_m.astype(f32))).astype(x.dtype)

    mix = jnp.concatenate([att, mls], axis=-1)
    return x + jnp.einsum("bse,ed->bsd", mix, w_out.astype(mix.dtype))


def setup_inputs(seed: int = 0) -> dict:
    key = jax.random.key(seed)
    ks = jax.random.split(key, 14)
    f32 = jnp.float32
    nrm = lambda k, shape, scale: jax.random.normal(k, shape, f32) * scale
    nblk = D_MLSTM // QKV_BLOCK
    return {
        "x": nrm(ks[0], (BATCH, SEQ, D_MODEL), 1.0),
        "norm_g": 1.0 + nrm(ks[1], (DEPTH, D_MODEL), 0.02),
        "w_in": nrm(ks[2], (DEPTH, D_MODEL, D_IN), D_MODEL ** -0.5),
        "conv_w": nrm(ks[3], (DEPTH, CONV_WIDTH, D_MLSTM), CONV_WIDTH ** -0.5),
        "conv_b": nrm(ks[4], (DEPTH, D_MLSTM), 0.02),
        "w_qm": nrm(ks[5], (DEPTH, nblk, QKV_BLOCK, QKV_BLOCK), QKV_BLOCK ** -0.5),
        "w_km": nrm(ks[6], (DEPTH, nblk, QKV_BLOCK, QKV_BLOCK), QKV_BLOCK ** -0.5),
        "w_vm": nrm(ks[7], (DEPTH, nblk, QKV_BLOCK, QKV_BLOCK), QKV_BLOCK ** -0.5),
        "w_if": nrm(ks[8], (DEPTH, 3 * D_MLSTM, 2 * MLSTM_HEADS), (3 * D_MLSTM) ** -0.5),
        "b_i": nrm(ks[9], (DEPTH, MLSTM_HEADS), 0.1),
        "b_f": jnp.linspace(3.0, 6.0, MLSTM_HEADS, dtype=f32)[None] + nrm(ks[10], (DEPTH, MLSTM_HEADS), 0.1),
        "hn_g": 1.0 + nrm(ks[11], (DEPTH, D_MLSTM), 0.02),
        "w_out": nrm(ks[12], (DEPTH, D_MIX, D_MODEL), D_MIX ** -0.5),
        "final_g": 1.0 + nrm(ks[13], (D_MODEL,), 0.02),
    }


def reference(x, norm_g, w_in, conv_w, conv_b, w_qm, w_km, w_vm, w_if, b_i, b_f, hn_g, w_out, final_g):
    for l in range(DEPTH):
        x = hybrid_layer(x, norm_g[l], w_in[l], conv_w[l], conv_b[l], w_qm[l], w_km[l], w_vm[l],
                         w_if[l], b_i[l], b_f[l], hn_g[l], w_out[l])
    return rmsnorm(x, final_g)
```

```python
import math
import os
import numpy as np
import ml_dtypes
import concourse.bass as bass
import concourse.mybir as mybir
from concourse.bass_utils import run_bass_kernel_spmd

F32 = mybir.dt.float32
BF16 = mybir.dt.bfloat16
AF = mybir.ActivationFunctionType
ALU = mybir.AluOpType
AX = mybir.AxisListType

ENGS = ["pe", "act", "dve", "pool", "sp"]
S = 4096
D = 1024
DIN = 7168
NSEQ = 2
DEPTH = 2
EPS = 1e-6
NEG = -30000.0
DILS = (1, 4, 16)


class Res:
    __slots__ = ("name", "writers", "readers", "excl")

    def __init__(self, name="", excl=False):
        self.name = name
        self.writers = {}
        self.readers = {}
        self.excl = excl


class Op:
    __slots__ = ("eng", "fn", "deps", "dma", "pos", "qidx", "needs_inc", "sig", "waits")


class Prog:
    def __init__(self, nc, K=8):
        self.nc = nc
        self.K = K
        self.ops = {e: [] for e in ENGS}
        self.ndma = {e: 0 for e in ENGS}
        self.pending = {e: [] for e in ENGS}

    def add(self, eng, fn, reads=(), writes=(), dma=False):
        o = Op()
        o.eng, o.fn, o.dma = eng, fn, dma
        o.pos = len(self.ops[eng])
        o.needs_inc = False
        o.sig = None
        o.waits = []
        o.qidx = -1
        if dma:
            o.qidx = self.ndma[eng]
            self.ndma[eng] += 1
        deps = []
        for r in reads:
            deps.extend(r.writers.values())
            if r.excl:
                deps.extend(o2 for o2 in r.readers.values() if o2.eng != eng)
        for w in writes:
            deps.extend(w.readers.values())
            deps.extend(w.writers.values())
        if self.pending[eng]:
            deps.extend(self.pending[eng])
            self.pending[eng] = []
        o.deps = deps
        k = (eng, o.qidx % self.K) if dma else (eng,)
        for r in reads:
            r.readers[k] = o
        for w in writes:
            w.writers[k] = o
        self.ops[eng].append(o)
        return o

    def barrier(self):
        last = []
        for e in ENGS:
            ops = self.ops[e]
            if not ops:
                continue
            last.append(ops[-1])
            seen = set()
            for o in reversed(ops):
                if o.dma:
                    s = o.qidx % self.K
                    if s not in seen:
                        seen.add(s)
                        last.append(o)
                    if len(seen) == self.K:
                        break
        for e in ENGS:
            self.pending[e] = list(last)

    def emit(self):
        nc = self.nc
        K = self.K
        from contextlib import ExitStack
        with ExitStack() as st:
            esem = {e: st.enter_context(nc.semaphore("s_" + e)) for e in ENGS}
            rsem = {e: [st.enter_context(nc.semaphore("r_%s%d" % (e, i))) for i in range(K)]
                    for e in ENGS if self.ndma[e] > 0}
            for e in ENGS:
                known = {}
                for o in self.ops[e]:
                    need = {}
                    for d in o.deps:
                        if d is o:
                            continue
                        if d.dma:
                            key = ("d", d.eng, d.qidx % K)
                            val = d.qidx // K + 1
                        else:
                            if d.eng == e and e == "pe":
                                continue
                            key = ("c", d.eng)
                            val = d.pos
                        if key not in need or need[key][0] < val:
                            need[key] = (val, d)
                    if o.dma and o.qidx >= K:
                        key = ("d", e, o.qidx % K)
                        val = o.qidx // K
                        if key not in need or need[key][0] < val:
                            need[key] = (val, None)
                    for key, (val, d) in need.items():
                        if known.get(key, -1) >= val:
                            continue
                        known[key] = val
                        if key[0] == "c":
                            d.needs_inc = True
                            o.waits.append(("c", d))
                        else:
                            o.waits.append(("d", key[1], key[2], val))
            for e in ENGS:
                cnt = 0
                for o in self.ops[e]:
                    if (not o.dma) and o.needs_inc:
                        cnt += 1
                        o.sig = cnt
            final = []
            for e in rsem:
                n = self.ndma[e]
                for s in range(K):
                    c = (n - s + K - 1) // K if n > s else 0
                    if c > 0:
                        final.append((rsem[e][s], 16 * c))

            def run(e, eng):
                for o in self.ops[e]:
                    for w in o.waits:
                        if w[0] == "c":
                            eng.wait_ge(esem[w[1].eng], w[1].sig)
                        else:
                            eng.wait_ge(rsem[w[1]][w[2]], 16 * w[3])
                    ins = o.fn(eng)
                    if o.dma:
                        ins.then_inc(rsem[e][o.qidx % K], 16)
                    elif o.needs_inc:
                        ins.then_inc(esem[e], 1)
                if e == "sp":
                    for sem, v in final:
                        eng.wait_ge(sem, v)

            with nc.Block() as block:
                @block.tensor
                def _(pe):
                    run("pe", pe)

                @block.scalar
                def _(act):
                    run("act", act)

                @block.vector
                def _(dve):
                    run("dve", dve)

                @block.gpsimd
                def _(pool):
                    run("pool", pool)

                @block.sync
                def _(sp):
                    run("sp", sp)


class T:
    __slots__ = ("ap", "res")

    def __init__(self, ap, name="", excl=False):
        self.ap = ap
        self.res = Res(name, excl)

    def __getitem__(self, k):
        return self.ap[k]


def build(layers=(0, 1), seqs=(0, 1), phases="ABCDE", dbg=False):
    nc = bass.Bass("TRN2", target_bir_lowering=False)
    p = Prog(nc)
    kind_dbg = "ExternalOutput" if dbg else "Internal"

    def din(name, shape, dt=F32):
        return nc.dram_tensor(name, list(shape), dt, kind="ExternalInput").ap()

    x_d = din("x", [NSEQ, S, D])
    normg_d = din("norm_g", [DEPTH, D])
    win_d = din("w_in", [DEPTH, D, DIN])
    cdiag_d = din("cdiag", [DEPTH, 128, 8, 4, 128])
    cb_d = din("cb", [DEPTH, 128, 8])
    wq_d = din("wq_bd", [DEPTH, 128, 8, 128])
    wk_d = din("wk_bd", [DEPTH, 128, 8, 128])
    wv_d = din("wv_bd", [DEPTH, 128, 8, 128])
    wif_d = din("w_if", [DEPTH, 128, 24, 32])
    bi_d = din("b_i", [DEPTH, 128, 4])
    bf_d = din("b_f", [DEPTH, 128, 4])
    hng_d = din("hn_g", [DEPTH, D])
    wout_d = din("w_out", [DEPTH, 2 * D, D])
    fing_d = din("final_g", [D])
    ident_d = din("ident", [128, 128], BF16)
    tri_d = din("tri", [128, 128])
    amask_d = din("amask", [128, 48, 128], BF16)
    y_d = nc.dram_tensor("y", [NSEQ, S, D], F32, kind="ExternalOutput").ap()

    def dscr(name, shape, dt=BF16):
        return nc.dram_tensor(name, list(shape), dt, kind=kind_dbg).ap()

    qT_d = dscr("qT_s", [NSEQ, 8, 128, S])
    kT_d = dscr("kT_s", [NSEQ, 8, 128, S])
    szT_d = dscr("szT_s", [NSEQ, 8, 128, S])
    v3_d = dscr("v3_s", [NSEQ, 3, 8, 128, 32, 128])
    xmT_d = dscr("xmT_s", [NSEQ, 8, 128, 8 + S])
    som_d = dscr("som_s", [NSEQ, S, D])
    szm_d = dscr("szm_s", [NSEQ, S, D])
    mixT_d = dscr("mixT_s", [NSEQ, 16, 128, S])
    qmT_d = dscr("qmT_s", [NSEQ, 8, 128, S])
    kmT_d = dscr("kmT_s", [NSEQ, 8, 128, S])
    km_d = dscr("km_s", [NSEQ, S, D])
    vm_d = dscr("vm_s", [NSEQ, S, D])
    x1_d = dscr("x1_s", [NSEQ, S, D], F32)
    gp_dbg = dscr("gp_s", [NSEQ, 128, 4, 128], F32) if dbg else None

    dres = {}

    def DR(*key):
        if key not in dres:
            dres[key] = Res(str(key))
        return dres[key]

    def persist(name, shape, dt):
        return T(nc.alloc_sbuf_tensor(name, list(shape), dt), name)

    ident = persist("ident_sb", [128, 128], BF16)
    tri = persist("tri_sb", [128, 128], F32)
    ones_f = persist("ones_f", [128, 128], F32)
    ones_b = persist("ones_b", [128, 128], BF16)
    tri_b = persist("tri_b", [128, 128], BF16)
    amask = persist("amask_sb", [128, 48, 128], BF16)
    cst = persist("cst", [128, 8], F32)
    gp = persist("gp", [128, 4, 128], F32)
    ARENA_W = 47 * 1024
    arena = nc.alloc_sbuf_tensor("arena", [128, ARENA_W], F32)
    banks = [nc.alloc_psum_tensor("bank%d" % i, [128, 512], F32) for i in range(8)]

    class Arena:
        def __init__(self):
            self.off = 0

        def get(self, name, shape, dt):
            n = 1
            for s_ in shape[1:]:
                n *= s_
            nbytes = n * (4 if dt == F32 else 2)
            nbytes = (nbytes + 63) // 64 * 64
            v = arena[:, self.off // 4:(self.off + nbytes) // 4]
            if dt != F32:
                v = v.bitcast(dt)
            v = v[:, 0:n]
            if len(shape) == 3:
                v = v.rearrange("p (a b) -> p a b", a=shape[1])
            elif len(shape) == 4:
                v = v.rearrange("p (a b c) -> p a b c", a=shape[1], b=shape[2])
            self.off += nbytes
            assert self.off <= ARENA_W * 4, (name, self.off)
            return T(v, name)

    def psum_views(dt=F32):
        out = []
        for b_ in banks:
            v = b_[:, :]
            if dt != F32:
                v = v.bitcast(dt)
            out.append(T(v, "bank", True))
        return out

    def dma(q, out, in_, reads=(), writes=()):
        p.add(q, lambda e: e.dma_start(out=out, in_=in_), reads=reads, writes=writes, dma=True)

    def mm(out, lhsT, rhs, start, stop, reads, writes, skip=False):
        if skip:
            p.add("pe", lambda e: e.matmul(out, lhsT=lhsT, rhs=rhs, start=start, stop=stop,
                                           skip_group_check=True), reads=reads, writes=writes)
        else:
            p.add("pe", lambda e: e.matmul(out, lhsT=lhsT, rhs=rhs, start=start, stop=stop),
                  reads=reads, writes=writes)

    def act(out, in_, func, reads, writes, bias=None, scale=1.0, accum=None):
        kw = {}
        if bias is not None:
            kw["bias"] = bias
        if accum is not None:
            kw["accum_out"] = accum
        p.add("act", lambda e: e.activation(out=out, in_=in_, func=func, scale=scale, **kw),
              reads=reads, writes=writes)

    def dve(fn, reads, writes):
        p.add("dve", fn, reads=reads, writes=writes)

    dma("sp", ident[:], ident_d, writes=[ident.res])
    dma("sp", tri[:], tri_d, writes=[tri.res])
    dma("sp", amask[:], amask_d, writes=[amask.res])
    dve(lambda e: e.memset(ones_f[:], 1.0), [], [ones_f.res])
    dve(lambda e: e.memset(ones_b[:], 1.0), [], [ones_b.res])
    dve(lambda e: e.tensor_copy(out=tri_b[:], in_=tri[:]), [tri.res], [tri_b.res])
    dve(lambda e: e.memset(cst[:, 0:1], EPS), [], [cst.res])
    dve(lambda e: e.memset(cst[:, 1:2], -math.log(16.0)), [], [cst.res])
    dve(lambda e: e.memset(cst[:, 2:3], 1.0), [], [cst.res])

    zpad = persist("zpad", [128, 8, 8], BF16)
    dve(lambda e: e.memset(zpad[:], 0.0), [], [zpad.res])
    for b_ in range(NSEQ):
        dma("sp", xmT_d[b_, :, :, 0:8].rearrange("c p t -> p c t"), zpad[:], reads=[zpad.res], writes=[DR("xmTpad", b_)])

    evac_rr = [0]

    def copy_evac(out, in_, reads, writes, scale=None):
        evac_rr[0] ^= 1
        if evac_rr[0]:
            act(out, in_, AF.Copy, reads, writes, scale=(1.0 if scale is None else scale))
        else:
            if scale is None:
                dve(lambda e: e.tensor_copy(out=out, in_=in_), reads, writes)
            else:
                dve(lambda e: e.tensor_scalar_mul(out=out, in0=in_, scalar1=scale), reads, writes)

    def phase_A(L, b):
        ar = Arena()
        hT = ar.get("hT", [128, 8, S], BF16)
        hres = [Res("hT%d" % i) for i in range(8)]
        xin = [ar.get("xin%d" % i, [128, 4, D], F32) for i in range(2)]
        xn = [ar.get("xn%d" % i, [128, D], BF16) for i in range(2)]
        junk = ar.get("junk", [128, D], BF16)
        W = [ar.get("W%d" % i, [128, 8, 512], BF16) for i in range(2)]
        ofm = [ar.get("ofm%d" % i, [128, S], BF16) for i in range(2)]
        otm = [ar.get("otm%d" % i, [128, 4, 512], BF16) for i in range(2)]
        vst = [ar.get("vst%d" % i, [128, 4, 32, 128], BF16) for i in range(1)]
        gn = ar.get("gn", [128, D], F32)
        stt = ar.get("stt", [128, 3, 32], F32)
        ps = psum_views()
        psT = [T(banks[i][:, :].bitcast(BF16), "psT", True) for i in range(2)]

        src = x_d if L == 0 else x1_d
        dma("sp", gn[:], normg_d[L].partition_broadcast(128), writes=[gn.res])

        def load_x(tq):
            dma("sp", xin[tq % 2][:], src[b, tq * 512:(tq + 1) * 512, :].rearrange("(n p) c -> p n c", p=128),
                reads=[DR("x1", b, tq)] if L > 0 else [], writes=[xin[tq % 2].res])

        jobs = []
        for wb in (0, 1):
            jobs.append(("fm", wb, AF.Copy, None))
        for wb in (2, 3):
            jobs.append(("fm", wb, AF.Copy, None))
        for wb in (8, 9):
            jobs.append(("fm", wb, AF.Copy, None))
        for di in range(3):
            for wb in (4, 5):
                jobs.append(("v", wb, AF.Copy, di))
        for wb in (6, 7):
            jobs.append(("fm", wb, AF.Silu, None))
        for wb in (12, 13):
            jobs.append(("tm", wb, AF.Silu, None))
        for wb in (10, 11):
            jobs.append(("tm", wb, AF.Sigmoid, None))
        wl = []
        for jb in jobs:
            if not wl or wl[-1] != jb[1]:
                wl.append(jb[1])
        wl_state = {"next": 0}

        def load_w():
            i = wl_state["next"]
            if i >= len(wl):
                return
            wb = wl[i]
            dma("pool", W[i % 2][:], win_d[L, :, wb * 512:(wb + 1) * 512].rearrange("(kc p) j -> p kc j", p=128),
                writes=[W[i % 2].res])
            wl_state["next"] = i + 1

        load_x(0)
        load_w()
        for tq in range(8):
            if tq + 1 < 8:
                load_x(tq + 1)
            xi = xin[tq % 2]
            for n in range(4):
                ti = tq * 4 + n
                xb = xn[ti % 2]
                act(junk[:], xi[:, n, :], AF.Square, [xi.res], [junk.res, stt.res], accum=stt[:, 0, ti:ti + 1])
                act(stt[:, 1, ti:ti + 1], stt[:, 0, ti:ti + 1], AF.Sqrt, [stt.res, cst.res], [stt.res],
                    bias=cst[:, 0:1], scale=1.0 / D)
                dve(lambda e, ti=ti: e.reciprocal(out=stt[:, 2, ti:ti + 1], in_=stt[:, 1, ti:ti + 1]),
                    [stt.res], [stt.res])
                dve(lambda e, xi=xi, n=n, ti=ti, xb=xb: e.scalar_tensor_tensor(
                    out=xb[:], in0=xi[:, n, :], scalar=stt[:, 2, ti:ti + 1], in1=gn[:],
                    op0=ALU.mult, op1=ALU.mult), [xi.res, stt.res, gn.res], [xb.res])
                pt = psT[ti % 2]
                for kc in range(8):
                    p.add("pe", lambda e, pt=pt, xb=xb, kc=kc: e.transpose(
                        pt[:, kc * 128:(kc + 1) * 128], xb[:, kc * 128:(kc + 1) * 128], ident[:]),
                        reads=[xb.res, ident.res], writes=[pt.res])
                copy_evac(hT[:, :, ti * 128:(ti + 1) * 128],
                          pt[:, :].rearrange("p (k t) -> p k t", k=8), [pt.res], [hres[tq]])
        pb = [2]

        def next_bank():
            r = ps[pb[0]]
            pb[0] = pb[0] + 1 if pb[0] < 7 else 2
            return r

        fm_i = [0]
        tm_i = [0]
        wcur = -1
        wprev = None
        for (kind, wb, func, di) in jobs:
            if wprev != wb:
                wcur += 1
                wprev = wb
                load_w()
            Wt = W[wcur % 2]
            if kind == "fm":
                for g in range(4):
                    col = wb * 512 + g * 128
                    if col < 1024:
                        dst, scale = qT_d[b, col // 128], 128 ** -0.5
                        dr = DR("qT", b, col // 128)
                    elif col < 2048:
                        dst, scale = kT_d[b, (col - 1024) // 128], None
                        dr = DR("kT", b, (col - 1024) // 128)
                    elif col < 4096:
                        dst, scale = szT_d[b, (col - 3072) // 128], None
                        dr = DR("szT", b, (col - 3072) // 128)
                    else:
                        dst, scale = xmT_d[b, (col - 4096) // 128][:, 8:8 + S], None
                        dr = DR("xmT", b, (col - 4096) // 128)
                    ob = ofm[fm_i[0] % 2]
                    fm_i[0] += 1
                    for tb in range(8):
                        bk = next_bank()
                        for kc in range(8):
                            mm(bk[:, :], Wt[:, kc, g * 128:(g + 1) * 128], hT[:, kc, tb * 512:(tb + 1) * 512],
                               kc == 0, kc == 7, [Wt.res, hres[tb]], [bk.res])
                        if func == AF.Copy:
                            copy_evac(ob[:, tb * 512:(tb + 1) * 512], bk[:, :], [bk.res], [ob.res], scale=scale)
                        else:
                            act(ob[:, tb * 512:(tb + 1) * 512], bk[:, :], func, [bk.res], [ob.res])
                    dma("sp", dst, ob[:], reads=[ob.res], writes=[dr])
            elif kind == "tm":
                dst_t, nm = (szm_d, "szm") if wb >= 12 else (som_d, "som")
                c0 = (wb % 2) * 512
                for ti in range(32):
                    ob = otm[tm_i[0] % 2]
                    bk = next_bank()
                    for kc in range(8):
                        mm(bk[:, :], hT[:, kc, ti * 128:(ti + 1) * 128], Wt[:, kc, :], kc == 0, kc == 7,
                           [Wt.res, hres[ti // 4]], [bk.res])
                    act(ob[:, ti % 4, :], bk[:, :], func, [bk.res], [ob.res])
                    if ti % 4 == 3:
                        tq = ti // 4
                        dma("sp", dst_t[b, tq * 512:(tq + 1) * 512, c0:c0 + 512].rearrange("(n p) c -> p n c", p=128),
                            ob[:], reads=[ob.res], writes=[DR(nm, b, tq)])
                        tm_i[0] += 1
            else:
                d_ = DILS[di]
                hh0 = (wb % 2) * 4
                vs = vst[0]
                for blk in range(32):
                    nblk = 32 // d_
                    r, n = blk // nblk, blk % nblk
                    base = r + d_ * 128 * n
                    bk = next_bank()
                    for kc in range(8):
                        mm(bk[:, :], hT[:, kc, base:base + d_ * 127 + 1:d_], Wt[:, kc, :], kc == 0, kc == 7,
                           [Wt.res] + [hres[i] for i in range(base // 512, min(8, (base + d_ * 128 - 1) // 512 + 1))],
                           [bk.res])
                    copy_evac(vs[:, :, blk, :], bk[:, :].rearrange("p (h c) -> p h c", h=4), [bk.res], [vs.res])
                for hh in range(4):
                    dma("sp", v3_d[b, di, hh0 + hh], vs[:, hh, :, :], reads=[vs.res], writes=[DR("v3", b, hh0 + hh)])

    def phase_B(L, b):
        ar = Arena()
        qT = [ar.get("qT%d" % i, [128, S], BF16) for i in range(2)]
        kT = [ar.get("kT%d" % i, [128, S], BF16) for i in range(2)]
        szT = [ar.get("szT%d" % i, [128, S], BF16) for i in range(2)]
        v3 = [[ar.get("v%d_%d" % (di, i), [128, 32, 128], BF16) for di in range(3)] for i in range(2)]
        accn = ar.get("accn", [128, S], F32)
        accd = ar.get("accd", [128, S], F32)
        att = [ar.get("att%d" % i, [128, S], BF16) for i in range(2)]
        PT = [ar.get("PT%d" % i, [128, 256], BF16) for i in range(4)]
        t1 = [ar.get("t1_%d" % i, [128, 512], F32) for i in range(2)]
        t2 = [ar.get("t2_%d" % i, [128, 512], F32) for i in range(2)]
        ps = psum_views()
        sc_i = [0]
        acc_i = [0]

        def load_head(h):
            i = h % 2
            dma("sp", qT[i][:], qT_d[b, h], reads=[DR("qT", b, h)], writes=[qT[i].res])
            dma("sp", kT[i][:], kT_d[b, h], reads=[DR("kT", b, h)], writes=[kT[i].res])
            dma("sp", szT[i][:], szT_d[b, h], reads=[DR("szT", b, h)], writes=[szT[i].res])
            for di in range(3):
                dma("sp", v3[i][di][:], v3_d[b, di, h], reads=[DR("v3", b, h)], writes=[v3[i][di].res])

        def qblock(h, di, qsl, ksl_prev, ksl_cur, blk_prev, blk_cur, outn, outd, first, last):
            i = h % 2
            sb = ps[sc_i[0] % 4]
            pt = PT[sc_i[0] % 4]
            sc_i[0] += 1
            mi = (h * 3 + di) * 2
            c0 = 0 if ksl_prev is not None else 128
            if ksl_prev is not None:
                mm(sb[:, 0:128], kT[i][:, ksl_prev], qT[i][:, qsl], True, False, [kT[i].res, qT[i].res], [sb.res])
                mm(sb[:, 0:128], ident[:], amask[:, mi + 1, :], False, True, [ident.res, amask.res], [sb.res])
            mm(sb[:, 128:256], kT[i][:, ksl_cur], qT[i][:, qsl], True, False, [kT[i].res, qT[i].res], [sb.res])
            mm(sb[:, 128:256], ident[:], amask[:, mi, :], False, True, [ident.res, amask.res], [sb.res])
            act(pt[:, c0:256], sb[:, c0:256], AF.Exp, [sb.res], [pt.res])
            vv = v3[i][di]
            for (o_, lw) in ((outn, None), (outd, ones_b)):
                seq = []
                if ksl_prev is not None:
                    seq.append((blk_prev, pt[:, 0:128]))
                seq.append((blk_cur, pt[:, 128:256]))
                for si, (bk_, rhs) in enumerate(seq):
                    lhs = vv[:, bk_, :] if lw is None else lw[:]
                    st_ = first and si == 0
                    sp_ = last and si == len(seq) - 1
                    mm(o_[0], lhs, rhs, st_, sp_, [vv.res if lw is None else lw.res, pt.res], [o_[1]], skip=True)

        load_head(0)
        for h in range(8):
            if h + 1 < 8:
                load_head(h + 1)
            i = h % 2
            for n in range(2):
                for rq in range(4):
                    bn = ps[4 + acc_i[0] % 2]
                    bd = ps[6 + acc_i[0] % 2]
                    acc_i[0] += 1
                    for rr in range(4):
                        r = rq * 4 + rr
                        qsl = slice(n * 2048 + r, n * 2048 + r + 16 * 127 + 1, 16)
                        kprev = slice((n - 1) * 2048 + r, (n - 1) * 2048 + r + 16 * 127 + 1, 16) if n >= 1 else None
                        qblock(h, 2, qsl, kprev, qsl, r * 2 + n - 1, r * 2 + n,
                               (bn[:, rr * 128:(rr + 1) * 128], bn.res), (bd[:, rr * 128:(rr + 1) * 128], bd.res),
                               True, True)
                    for (bk_, acc, eng) in ((bn, accn, "act"), (bd, accd, "dve")):
                        dst = acc[:, n * 2048:(n + 1) * 2048].rearrange("p (j r) -> p j r", r=16)[:, :, rq * 4:rq * 4 + 4]
                        dst = dst.rearrange("p j r -> p r j")
                        srcv = bk_[:, :].rearrange("p (r j) -> p r j", r=4)
                        if eng == "act":
                            act(dst, srcv, AF.Copy, [bk_.res], [acc.res])
                        else:
                            dve(lambda e, dst=dst, srcv=srcv: e.tensor_copy(out=dst, in_=srcv), [bk_.res], [acc.res])
            for tb in range(8):
                bn = ps[4 + acc_i[0] % 2]
                bd = ps[6 + acc_i[0] % 2]
                acc_i[0] += 1
                for qb in range(4):
                    n = tb * 4 + qb
                    qsl = slice(n * 128, (n + 1) * 128)
                    kprev = slice((n - 1) * 128, n * 128) if n >= 1 else None
                    qblock(h, 0, qsl, kprev, qsl, n - 1, n,
                           (bn[:, qb * 128:(qb + 1) * 128], bn.res), (bd[:, qb * 128:(qb + 1) * 128], bd.res),
                           qb == 0, False)
                for r in range(4):
                    qsl = slice(tb * 512 + r, tb * 512 + r + 4 * 127 + 1, 4)
                    kprev = slice((tb - 1) * 512 + r, (tb - 1) * 512 + r + 4 * 127 + 1, 4) if tb >= 1 else None
                    qblock(h, 1, qsl, kprev, qsl, r * 8 + tb - 1, r * 8 + tb,
                           (bn[:, r:r + 4 * 127 + 1:4], bn.res), (bd[:, r:r + 4 * 127 + 1:4], bd.res), False, r == 3)
                a1, a2 = t1[tb % 2], t2[tb % 2]
                cs = slice(tb * 512, (tb + 1) * 512)
                dve(lambda e, a1=a1, bn=bn, cs=cs: e.tensor_tensor(out=a1[:], in0=bn[:, :], in1=accn[:, cs], op=ALU.add),
                    [bn.res, accn.res], [a1.res])
                dve(lambda e, a2=a2, bd=bd, cs=cs: e.tensor_tensor(out=a2[:], in0=bd[:, :], in1=accd[:, cs], op=ALU.add),
                    [bd.res, accd.res], [a2.res])
                dve(lambda e, a2=a2: e.reciprocal(out=a2[:], in_=a2[:]), [a2.res], [a2.res])
                dve(lambda e, a1=a1, a2=a2: e.tensor_tensor(out=a1[:], in0=a1[:], in1=a2[:], op=ALU.mult),
                    [a1.res, a2.res], [a1.res])
                dve(lambda e, a1=a1, i=i, cs=cs: e.tensor_tensor(out=att[i][:, cs], in0=a1[:], in1=szT[i][:, cs], op=ALU.mult),
                    [a1.res, szT[i].res], [att[i].res])
            dma("sp", mixT_d[b, h], att[i][:], reads=[att[i].res], writes=[DR("mixT", b, h)])

    def phase_C(L, b):
        ar = Arena()
        xf = [ar.get("xf%d" % i, [128, 8, 520], BF16) for i in range(2)]
        xc = [ar.get("xc%d" % i, [128, 8, 512], BF16) for i in range(2)]
        qs = [ar.get("qs%d" % i, [128, 8, 512], BF16) for i in range(2)]
        ks = [ar.get("ks%d" % i, [128, 8, 512], BF16) for i in range(2)]
        vs = [ar.get("vs%d" % i, [128, 8, 512], BF16) for i in range(2)]
        ktm = [ar.get("ktm%d" % i, [128, 4, D], BF16) for i in range(2)]
        vtm = [ar.get("vtm%d" % i, [128, 4, D], BF16) for i in range(2)]
        cdg = ar.get("cdg", [128, 8, 4, 128], BF16)
        wq = ar.get("wq", [128, 8, 128], BF16)
        wk = ar.get("wk", [128, 8, 128], BF16)
        wv = ar.get("wv", [128, 8, 128], BF16)
        wif = ar.get("wif", [128, 24, 32], BF16)
        cb = ar.get("cb", [128, 8], F32)
        bib = ar.get("bib", [128, 4], F32)
        bfb = ar.get("bfb", [128, 4], F32)
        graw = ar.get("graw", [128, 32, 8], F32)
        gt = ar.get("gt", [128, 4, 128], F32)
        xh = ar.get("xh", [128, 3, 128], BF16)
        xr_ = ar.get("xr_", [128, 2, 128], F32)
        ps = psum_views()

        for c_ in range(8):
            dma("pool", cdg[:, c_, :, :], cdiag_d[L, :, c_, :, :], writes=[cdg.res])
        dma("pool", wq[:], wq_d[L], writes=[wq.res])
        dma("pool", wk[:], wk_d[L], writes=[wk.res])
        dma("pool", wv[:], wv_d[L], writes=[wv.res])
        dma("pool", wif[:], wif_d[L], writes=[wif.res])
        dma("sp", cb[:], cb_d[L], writes=[cb.res])
        dma("sp", bib[:], bi_d[L], writes=[bib.res])
        dma("sp", bfb[:], bf_d[L], writes=[bfb.res])
        dve(lambda e: e.tensor_scalar_mul(out=bfb[:], in0=bfb[:], scalar1=-1.0), [bfb.res], [bfb.res])

        def load(tb):
            i = tb % 2
            dma("sp", xf[i][:], xmT_d[b, :, :, tb * 512:tb * 512 + 520].rearrange("c p t -> p c t"),
                reads=[DR("xmT", b, c) for c in range(8)] + [DR("xmTpad", b)], writes=[xf[i].res])

        load(0)
        cv_i = [0]
        LV = int(os.environ.get("CBIS", "9"))
        for tb in range(8):
            i = tb % 2
            if tb + 1 < 8:
                load(tb + 1)
            for c in range(8 if LV >= 2 else 0):
                bk = ps[cv_i[0] % 2]
                cv_i[0] += 1
                for w in range(4):
                    mm(bk[:, :], cdg[:, c, w, :], xf[i][:, c, 5 + w:5 + w + 512], w == 0, w == 3, [cdg.res, xf[i].res], [bk.res])
                act(xc[i][:, c, :], bk[:, :], AF.Silu, [bk.res, cb.res], [xc[i].res], bias=cb[:, c:c + 1])
                if LV < 3:
                    continue
                for (wt, srcv, sres, dstt, bi_) in ((wq, xc[i][:, c, :], xc[i].res, qs[i], 2),
                                                   (wk, xc[i][:, c, :], xc[i].res, ks[i], 3),
                                                   (wv, xf[i][:, c, 8:520], xf[i].res, vs[i], 2)):
                    bk2 = ps[bi_] if wt is not wv else ps[2 + (c % 2)]
                    mm(bk2[:, :], wt[:, c, :], srcv, True, True, [wt.res, sres], [bk2.res])
                    copy_evac(dstt[:, c, :], bk2[:, :], [bk2.res], [dstt.res])
                if LV < 4:
                    continue
                for (wt, lsrc, off, dstt, bi_) in ((wk, xc[i], 0, ktm[i], 4), (wv, xf[i], 8, vtm[i], 5)):
                    bk3 = ps[bi_]
                    for ti in range(4):
                        mm(bk3[:, ti * 128:(ti + 1) * 128], lsrc[:, c, off + ti * 128:off + (ti + 1) * 128], wt[:, c, :],
                           True, True, [wt.res, lsrc.res], [bk3.res])
                    copy_evac(dstt[:, :, c * 128:(c + 1) * 128], bk3[:, :].rearrange("p (t c) -> p t c", t=4),
                              [bk3.res], [dstt.res])
            bg = ps[6]
            for ti in range(4 if LV >= 5 else 0):
                k_ = 0
                for (srct, w0) in ((qs[i], 0), (ks[i], 8), (vs[i], 16)):
                    for c in range(8):
                        mm(bg[:, ti * 32:(ti + 1) * 32], srct[:, c, ti * 128:(ti + 1) * 128], wif[:, w0 + c, :],
                           k_ == 0, k_ == 23, [srct.res, wif.res], [bg.res])
                        k_ += 1
            dve(lambda e, tb=tb, bg=bg: e.tensor_copy(out=graw[:, tb * 4:(tb + 1) * 4, :],
                                                      in_=bg[:, 0:128].rearrange("p (t g) -> p t g", t=4)[:, :, 0:8]),
                [bg.res], [graw.res])
            dma("sp", qmT_d[b, :, :, tb * 512:(tb + 1) * 512].rearrange("c p t -> p c t"), qs[i][:],
                reads=[qs[i].res], writes=[DR("qmT", b, tb)])
            dma("sp", kmT_d[b, :, :, tb * 512:(tb + 1) * 512].rearrange("c p t -> p c t"), ks[i][:],
                reads=[ks[i].res], writes=[DR("kmT", b, tb)])
            dma("sp", km_d[b, tb * 512:(tb + 1) * 512, :].rearrange("(n p) c -> p n c", p=128), ktm[i][:],
                reads=[ktm[i].res], writes=[DR("km", b, tb)])
            dma("sp", vm_d[b, tb * 512:(tb + 1) * 512, :].rearrange("(n p) c -> p n c", p=128), vtm[i][:],
                reads=[vtm[i].res], writes=[DR("vm", b, tb)])
        if LV < 6:
            return
        li = gt[:, 0, :].rearrange("p (j h) -> p j h", h=4)
        ef = gt[:, 1, :].rearrange("p (j h) -> p j h", h=4)
        for hd in range(4):
            dve(lambda e, hd=hd: e.tensor_scalar_add(out=li[:, :, hd], in0=graw[:, :, hd], scalar1=bib[:, hd:hd + 1]),
                [graw.res, bib.res], [gt.res])
            act(ef[:, :, hd], graw[:, :, 4 + hd], AF.Exp, [graw.res, bfb.res], [gt.res], bias=bfb[:, hd:hd + 1], scale=-1.0)
        if LV < 7:
            return
        act(gt[:, 1, :], gt[:, 1, :], AF.Ln, [gt.res, cst.res], [gt.res], bias=cst[:, 2:3], scale=1.0)
        if LV < 8:
            return
        bc = ps[7]
        dve(lambda e: e.tensor_copy(out=xh[:, 0, :], in_=gt[:, 1, :]), [gt.res], [xh.res])
        dve(lambda e: e.tensor_tensor(out=xr_[:, 0, :], in0=gt[:, 1, :], in1=xh[:, 0, :], op=ALU.subtract),
            [gt.res, xh.res], [xr_.res])
        dve(lambda e: e.tensor_copy(out=xh[:, 1, :], in_=xr_[:, 0, :]), [xr_.res], [xh.res])
        dve(lambda e: e.tensor_tensor(out=xr_[:, 1, :], in0=xr_[:, 0, :], in1=xh[:, 1, :], op=ALU.subtract),
            [xr_.res, xh.res], [xr_.res])
        dve(lambda e: e.tensor_copy(out=xh[:, 2, :], in_=xr_[:, 1, :]), [xr_.res], [xh.res])
        for k_ in range(3):
            mm(bc[:, 0:128], tri_b[:], xh[:, k_, :], k_ == 0, k_ == 2, [tri_b.res, xh.res], [bc.res])
        for k_ in range(3):
            mm(bc[:, 128:256], ones_b[:], xh[:, k_, :], k_ == 0, k_ == 2, [ones_b.res, xh.res], [bc.res])
        if LV == 8:
            return
        act(gp[:, 0, :], bc[:, 0:128], AF.Exp, [bc.res, cst.res], [gp.res], bias=cst[:, 1:2], scale=-1.0)
        if LV == 10:
            return
        dve(lambda e: e.tensor_tensor(out=gt[:, 2, :], in0=bc[:, 0:128], in1=gt[:, 0, :], op=ALU.add),
            [bc.res, gt.res], [gt.res])
        act(gp[:, 1, :], gt[:, 2, :], AF.Exp, [gt.res], [gp.res])
        act(gp[:, 3, :], bc[:, 128:256], AF.Exp, [bc.res], [gp.res], scale=-1.0)
        if LV == 12:
            return
        dve(lambda e: e.tensor_tensor(out=gp[:, 2, :], in0=gp[:, 1, :], in1=gp[:, 3, :], op=ALU.mult),
            [gp.res], [gp.res])
        if LV == 13:
            return
        if dbg:
            dma("sp", gp_dbg[b], gp[:], reads=[gp.res])

    def phase_D(L, b):
        ar = Arena()
        qb_ = [ar.get("qb%d" % i, [128, 8, 512], BF16) for i in range(2)]
        kb_ = [ar.get("kb%d" % i, [128, 8, 512], BF16) for i in range(2)]
        ktm = [ar.get("ktm%d" % i, [128, 4, D], BF16) for i in range(2)]
        vtm = [ar.get("vtm%d" % i, [128, 4, D], BF16) for i in range(2)]
        som = [ar.get("som%d" % i, [128, 4, D], BF16) for i in range(2)]
        szm = [ar.get("szm%d" % i, [128, 4, D], BF16) for i in range(2)]
        C32 = ar.get("C32", [128, 4, 2, 260], F32)
        Cbf = ar.get("Cbf", [128, 4, 2, 260], BF16)
        Cres = [Res("C%d" % i) for i in range(4)]
        Cbres = [Res("Cb%d" % i) for i in range(4)]
        St = [ar.get("St%d" % i, [128, 128], BF16) for i in range(2)]
        vp = [ar.get("vp%d" % i, [128, 4, 260], BF16) for i in range(2)]
        vpp = [ar.get("vpp%d" % i, [128, 4, 260], BF16) for i in range(2)]
        hm = [ar.get("hm%d" % i, [128, D], F32) for i in range(2)]
        sq = ar.get("sq", [128, D], F32)
        gz = ar.get("gz", [128, D], F32)
        mls = [ar.get("mls%d" % i, [128, D], BF16) for i in range(2)]
        hng = ar.get("hng", [128, D], F32)
        mxs = [ar.get("mxs%d" % i, [128, 8, 512], BF16) for i in range(2)]
        sm = [ar.get("sm%d" % i, [128, 8, 4], F32) for i in range(2)]
        ds_ = [ar.get("ds%d" % i, [128, 4], F32) for i in range(4)]
        ps = psum_views()
        psTb = [T(banks[6 + i][:, :].bitcast(BF16), "psTb", True) for i in range(2)]

        dma("sp", hng[:], hng_d[L].partition_broadcast(128), writes=[hng.res])
        dve(lambda e: e.memset(C32[:, :, :, :], 0.0), [], Cres)
        dve(lambda e: e.memset(Cbf[:, :, :, :], 0.0), [], Cbres)

        def load(tb):
            i = tb % 2
            cs = slice(tb * 512, (tb + 1) * 512)
            dma("sp", qb_[i][:], qmT_d[b, :, :, cs].rearrange("c p t -> p c t"), reads=[DR("qmT", b, tb)], writes=[qb_[i].res])
            dma("sp", kb_[i][:], kmT_d[b, :, :, cs].rearrange("c p t -> p c t"), reads=[DR("kmT", b, tb)], writes=[kb_[i].res])
            for (t_, d_, nm) in ((ktm, km_d, "km"), (vtm, vm_d, "vm"), (som, som_d, "som"), (szm, szm_d, "szm")):
                dma("sp", t_[i][:], d_[b, cs, :].rearrange("(n p) c -> p n c", p=128), reads=[DR(nm, b, tb)], writes=[t_[i].res])

        load(0)
        cnt = [0]
        for tb in range(8):
            i = tb % 2
            if tb + 1 < 8:
                load(tb + 1)
            mx = mxs[i]
            for jj in range(4):
                j = tb * 4 + jj
                tk = slice(jj * 128, (jj + 1) * 128)
                vpi, vppi = vp[j % 2], vpp[j % 2]
                for hd in range(4):
                    g_ = j * 4 + hd
                    act(vpi[:, hd, 0:256], vtm[i][:, jj, hd * 256:(hd + 1) * 256], AF.Copy, [vtm[i].res, gp.res], [vpi.res],
                        scale=gp[:, 1, g_:g_ + 1])
                    act(vppi[:, hd, 0:256], vtm[i][:, jj, hd * 256:(hd + 1) * 256], AF.Copy, [vtm[i].res, gp.res], [vppi.res],
                        scale=gp[:, 2, g_:g_ + 1])
                p.add("pool", lambda e, vpi=vpi, j=j: e.tensor_copy(out=vpi[:, :, 256], in_=gp[:, 1, j * 4:(j + 1) * 4]),
                      reads=[gp.res], writes=[vpi.res])
                p.add("pool", lambda e, vppi=vppi, j=j: e.tensor_copy(out=vppi[:, :, 256], in_=gp[:, 2, j * 4:(j + 1) * 4]),
                      reads=[gp.res], writes=[vppi.res])
                hmi = hm[j % 2]
                dve(lambda e, i=i, jj=jj: e.tensor_tensor(out=gz[:], in0=hng[:], in1=szm[i][:, jj, :], op=ALU.mult),
                    [hng.res, szm[i].res], [gz.res])
                for hd in range(4):
                    g_ = j * 4 + hd
                    k_ = cnt[0]
                    cnt[0] += 1
                    bs = ps[k_ % 2]
                    bn = ps[2 + k_ % 2]
                    sti = St[k_ % 2]
                    dsi = ds_[k_ % 4]
                    for half in range(2):
                        mm(bs[:, 0:128], kb_[i][:, 2 * hd + half, tk], qb_[i][:, 2 * hd + half, tk], half == 0, half == 1,
                           [kb_[i].res, qb_[i].res], [bs.res])
                    dve(lambda e, sti=sti, bs=bs: e.tensor_tensor(out=sti[:], in0=bs[:, 0:128], in1=tri[:], op=ALU.mult),
                        [bs.res, tri.res], [sti.res])
                    for half in range(2):
                        mm(bn[:, 0:257], qb_[i][:, 2 * hd + half, tk], Cbf[:, hd, half, 0:257], half == 0, False,
                           [qb_[i].res, Cbres[hd]], [bn.res])
                    mm(bn[:, 0:257], sti[:], vpi[:, hd, 0:257], False, True, [sti.res, vpi.res], [bn.res])
                    for half in range(2):
                        bd = ps[4 + half]
                        mm(bd[:, 0:257], ktm[i][:, jj, hd * 256 + half * 128:hd * 256 + (half + 1) * 128], vppi[:, hd, 0:257],
                           True, True, [ktm[i].res, vppi.res], [bd.res])
                        dve(lambda e, hd=hd, half=half, bd=bd, g_=g_: e.scalar_tensor_tensor(
                            out=C32[:, hd, half, 0:257], in0=C32[:, hd, half, 0:257], scalar=gp[:, 3, g_:g_ + 1],
                            in1=bd[:, 0:257], op0=ALU.mult, op1=ALU.add), [Cres[hd], gp.res, bd.res], [Cres[hd]])
                    act(Cbf[:, hd, :, 0:257], C32[:, hd, :, 0:257], AF.Copy, [Cres[hd]], [Cbres[hd]])
                    act(dsi[:, 0:1], bn[:, 256:257], AF.Abs, [bn.res, gp.res], [dsi.res], scale=gp[:, 0, g_:g_ + 1])
                    dve(lambda e, dsi=dsi: e.tensor_scalar_max(out=dsi[:, 1:2], in0=dsi[:, 0:1], scalar1=1.0), [dsi.res], [dsi.res])
                    dve(lambda e, dsi=dsi: e.reciprocal(out=dsi[:, 2:3], in_=dsi[:, 1:2]), [dsi.res], [dsi.res])
                    dve(lambda e, dsi=dsi, g_=g_: e.tensor_tensor(out=dsi[:, 3:4], in0=dsi[:, 2:3], in1=gp[:, 0, g_:g_ + 1], op=ALU.mult),
                        [dsi.res, gp.res], [dsi.res])
                    dve(lambda e, hmi=hmi, hd=hd, bn=bn, dsi=dsi, i=i, jj=jj: e.scalar_tensor_tensor(
                        out=hmi[:, hd * 256:(hd + 1) * 256], in0=bn[:, 0:256], scalar=dsi[:, 3:4],
                        in1=som[i][:, jj, hd * 256:(hd + 1) * 256], op0=ALU.mult, op1=ALU.mult),
                        [bn.res, dsi.res, som[i].res], [hmi.res])
                smi = sm[j % 2]
                hv = hmi[:, :].rearrange("p (h d) -> p h d", h=4)
                dve(lambda e, smi=smi, hv=hv: e.reduce_sum(out=smi[:, 0, :], in_=hv, axis=AX.X), [hmi.res], [smi.res])
                act(sq[:], hmi[:], AF.Square, [hmi.res], [sq.res])
                dve(lambda e, smi=smi: e.reduce_sum(out=smi[:, 1, :], in_=sq[:, :].rearrange("p (h d) -> p h d", h=4), axis=AX.X),
                    [sq.res], [smi.res])
                dve(lambda e, smi=smi: e.tensor_scalar_mul(out=smi[:, 2, :], in0=smi[:, 0, :], scalar1=1.0 / 256), [smi.res], [smi.res])
                dve(lambda e, smi=smi: e.tensor_tensor(out=smi[:, 3, :], in0=smi[:, 2, :], in1=smi[:, 2, :], op=ALU.mult), [smi.res], [smi.res])
                dve(lambda e, smi=smi: e.scalar_tensor_tensor(out=smi[:, 4, :], in0=smi[:, 1, :], scalar=1.0 / 256, in1=smi[:, 3, :],
                                                              op0=ALU.mult, op1=ALU.subtract), [smi.res], [smi.res])
                act(smi[:, 5, :], smi[:, 4, :], AF.Sqrt, [smi.res, cst.res], [smi.res], bias=cst[:, 0:1], scale=1.0)
                dve(lambda e, smi=smi: e.reciprocal(out=smi[:, 6, :], in_=smi[:, 5, :]), [smi.res], [smi.res])
                for hd in range(4):
                    dve(lambda e, hd=hd, smi=smi, hmi=hmi: e.tensor_scalar(
                        out=hmi[:, hd * 256:(hd + 1) * 256], in0=hmi[:, hd * 256:(hd + 1) * 256],
                        scalar1=smi[:, 2, hd:hd + 1], scalar2=smi[:, 6, hd:hd + 1], op0=ALU.subtract, op1=ALU.mult),
                        [hmi.res, smi.res], [hmi.res])
                ml = mls[j % 2]
                dve(lambda e, ml=ml, hmi=hmi: e.tensor_tensor(out=ml[:], in0=hmi[:], in1=gz[:], op=ALU.mult),
                    [hmi.res, gz.res], [ml.res])
                pt = psTb[j % 2]
                for c in range(8):
                    p.add("pe", lambda e, pt=pt, ml=ml, c=c: e.transpose(pt[:, c * 128:(c + 1) * 128], ml[:, c * 128:(c + 1) * 128], ident[:]),
                          reads=[ml.res, ident.res], writes=[pt.res])
                act(mx[:, :, tk], pt[:, :].rearrange("p (c t) -> p c t", c=8), AF.Copy, [pt.res], [mx.res])
            dma("sp", mixT_d[b, 8:16, :, tb * 512:(tb + 1) * 512].rearrange("c p t -> p c t"), mx[:],
                reads=[mx.res], writes=[DR("mixT", b, 8 + tb)])

    def phase_E(L, b):
        ar = Arena()
        wo = ar.get("wo", [128, 16, D], BF16)
        mxl = [ar.get("mxl%d" % i, [128, 16, 512], BF16) for i in range(2)]
        xr = [ar.get("xr%d" % i, [128, 4, D], F32) for i in range(2)]
        xo = [ar.get("xo%d" % i, [128, 4, D], F32) for i in range(2)]
        gf = ar.get("gf", [128, D], F32)
        junk = ar.get("junkE", [128, D], F32)
        stt = ar.get("sttE", [128, 3, 32], F32)
        ps = psum_views()
        last = (L == DEPTH - 1)
        src = x_d if L == 0 else x1_d
        dma("pool", wo[:], wout_d[L].rearrange("(k p) c -> p k c", p=128), writes=[wo.res])
        if last:
            dma("sp", gf[:], fing_d.partition_broadcast(128), writes=[gf.res])

        def load(tb):
            i = tb % 2
            cs = slice(tb * 512, (tb + 1) * 512)
            dma("sp", mxl[i][:], mixT_d[b, :, :, cs].rearrange("c p t -> p c t"),
                reads=[DR("mixT", b, k_) for k_ in range(16)], writes=[mxl[i].res])
            dma("sp", xr[i][:], src[b, cs, :].rearrange("(n p) c -> p n c", p=128),
                reads=[DR("x1", b, tb)] if L > 0 else [], writes=[xr[i].res])

        load(0)
        k_ = 0
        for tb in range(8):
            i = tb % 2
            if tb + 1 < 8:
                load(tb + 1)
            for n in range(4):
                for half in range(2):
                    bk = ps[k_ % 4]
                    k_ += 1
                    for kc in range(16):
                        mm(bk[:, :], mxl[i][:, kc, n * 128:(n + 1) * 128], wo[:, kc, half * 512:(half + 1) * 512],
                           kc == 0, kc == 15, [mxl[i].res, wo.res], [bk.res])
                    dve(lambda e, i=i, n=n, half=half, bk=bk: e.tensor_tensor(
                        out=xo[i][:, n, half * 512:(half + 1) * 512], in0=bk[:, :],
                        in1=xr[i][:, n, half * 512:(half + 1) * 512], op=ALU.add), [bk.res, xr[i].res], [xo[i].res])
                if last:
                    ti = tb * 4 + n
                    act(junk[:], xo[i][:, n, :], AF.Square, [xo[i].res], [junk.res, stt.res], accum=stt[:, 0, ti:ti + 1])
                    act(stt[:, 1, ti:ti + 1], stt[:, 0, ti:ti + 1], AF.Sqrt, [stt.res, cst.res], [stt.res],
                        bias=cst[:, 0:1], scale=1.0 / D)
                    dve(lambda e, ti=ti: e.reciprocal(out=stt[:, 2, ti:ti + 1], in_=stt[:, 1, ti:ti + 1]), [stt.res], [stt.res])
                    dve(lambda e, i=i, n=n, ti=ti: e.scalar_tensor_tensor(
                        out=xo[i][:, n, :], in0=xo[i][:, n, :], scalar=stt[:, 2, ti:ti + 1], in1=gf[:],
                        op0=ALU.mult, op1=ALU.mult), [xo[i].res, stt.res, gf.res], [xo[i].res])
            dst = y_d if last else x1_d
            dma("sp", dst[b, tb * 512:(tb + 1) * 512, :].rearrange("(n p) c -> p n c", p=128), xo[i][:],
                reads=[xo[i].res], writes=[] if last else [DR("x1", b, tb)])

    fns = {"A": phase_A, "B": phase_B, "C": phase_C, "D": phase_D, "E": phase_E}
    for L in layers:
        for b in seqs:
            for ph in phases:
                fns[ph](L, b)
                p.barrier()
    p.emit()
    return nc


def host_consts():
    ident = np.eye(128, dtype=np.float32).astype(ml_dtypes.bfloat16)
    tri = np.triu(np.ones((128, 128), np.float32))
    slopes = np.exp2(-8.0 * np.arange(1, 9, dtype=np.float32) / 8.0)
    i = np.arange(128)[:, None]
    j = np.arange(128)[None, :]
    am = np.zeros((128, 48, 128), np.float32)
    for h in range(8):
        for di, d_ in enumerate(DILS):
            cur = np.where(j >= i, -slopes[h] * d_ * (j - i), NEG)
            prev = np.where(j <= i, -slopes[h] * d_ * (128 + j - i), NEG)
            am[:, (h * 3 + di) * 2, :] = cur
            am[:, (h * 3 + di) * 2 + 1, :] = prev
    return ident, tri, am.astype(ml_dtypes.bfloat16)


def host_layout(inputs):
    f = lambda a: np.ascontiguousarray(np.asarray(a, dtype=np.float32))
    conv_w = f(inputs["conv_w"])
    cdiag = np.zeros((DEPTH, 128, 8, 4, 128), np.float32)
    pidx = np.arange(128)
    for L in range(DEPTH):
        for c in range(8):
            for w in range(4):
                cdiag[L, pidx, c, w, pidx] = conv_w[L, w, c * 128 + pidx]
    cb = np.ascontiguousarray(f(inputs["conv_b"]).reshape(DEPTH, 8, 128).transpose(0, 2, 1))

    def bd(w):
        w = f(w)
        out = np.zeros((DEPTH, 128, 8, 128), np.float32)
        for L in range(DEPTH):
            for c in range(8):
                for n in range(32):
                    out[L, n * 4:(n + 1) * 4, c, n * 4:(n + 1) * 4] = w[L, c * 32 + n]
        return out

    ident, tri, am = host_consts()
    common = {
        "norm_g": f(inputs["norm_g"]), "w_in": f(inputs["w_in"]), "cdiag": cdiag, "cb": cb,
        "wq_bd": bd(inputs["w_qm"]), "wk_bd": bd(inputs["w_km"]), "wv_bd": bd(inputs["w_vm"]),
        "w_if": np.ascontiguousarray(np.pad(f(inputs["w_if"]).reshape(DEPTH, 24, 128, 8).transpose(0, 2, 1, 3), ((0, 0), (0, 0), (0, 0), (0, 24)))), "b_i": np.ascontiguousarray(np.broadcast_to(f(inputs["b_i"])[:, None, :], (DEPTH, 128, 4))), "b_f": np.ascontiguousarray(np.broadcast_to(f(inputs["b_f"])[:, None, :], (DEPTH, 128, 4))),
        "hn_g": f(inputs["hn_g"]), "w_out": f(inputs["w_out"]), "final_g": f(inputs["final_g"]),
        "ident": ident, "tri": tri, "amask": am,
    }
    return common


def kernel(**inputs):
    x = np.ascontiguousarray(np.asarray(inputs["x"], dtype=np.float32))
    common = host_layout(inputs)
    nc = build()
    in_maps = []
    for c in range(8):
        m = dict(common)
        m["x"] = x[c * NSEQ:(c + 1) * NSEQ]
        in_maps.append(m)
    res = run_bass_kernel_spmd(nc, in_maps, core_ids=list(range(8)))
    return np.concatenate([r["y"] for r in res.results], axis=0).astype(np.float32)
```

```python
import math
import os
import numpy as np
import ml_dtypes
import concourse.bass as bass
import concourse.mybir as mybir
from concourse.bass_utils import run_bass_kernel_spmd

F32 = mybir.dt.float32
BF16 = mybir.dt.bfloat16
AF = mybir.ActivationFunctionType
ALU = mybir.AluOpType
AX = mybir.AxisListType

ENGS = ["pe", "act", "dve", "pool", "sp"]
S = 4096
D = 1024
DIN = 7168
NSEQ = 2
DEPTH = 2
EPS = 1e-6
NEG = -30000.0
DILS = (1, 4, 16)


class Res:
    __slots__ = ("name", "writers", "readers", "excl")

    def __init__(self, name="", excl=False):
        self.name = name
        self.writers = {}
        self.readers = {}
        self.excl = excl


class Op:
    __slots__ = ("eng", "fn", "deps", "dma", "pos", "qidx", "needs_inc", "sig", "waits")


class Prog:
    def __init__(self, nc, K=8):
        self.nc = nc
        self.K = K
        self.ops = {e: [] for e in ENGS}
        self.ndma = {e: 0 for e in ENGS}
        self.pending = {e: [] for e in ENGS}

    def add(self, eng, fn, reads=(), writes=(), dma=False):
        o = Op()
        o.eng, o.fn, o.dma = eng, fn, dma
        o.pos = len(self.ops[eng])
        o.needs_inc = False
        o.sig = None
        o.waits = []
        o.qidx = -1
        if dma:
            o.qidx = self.ndma[eng]
            self.ndma[eng] += 1
        deps = []
        for r in reads:
            deps.extend(r.writers.values())
            if r.excl:
                deps.extend(o2 for o2 in r.readers.values() if o2.eng != eng)
        for w in writes:
            deps.extend(w.readers.values())
            deps.extend(w.writers.values())
        if self.pending[eng]:
            deps.extend(self.pending[eng])
            self.pending[eng] = []
        o.deps = deps
        k = (eng, o.qidx % self.K) if dma else (eng,)
        for r in reads:
            r.readers[k] = o
        for w in writes:
            w.writers[k] = o
        self.ops[eng].append(o)
        return o

    def barrier(self):
        last = []
        for e in ENGS:
            ops = self.ops[e]
            if not ops:
                continue
            last.append(ops[-1])
            seen = set()
            for o in reversed(ops):
                if o.dma:
                    s = o.qidx % self.K
                    if s not in seen:
                        seen.add(s)
                        last.append(o)
                    if len(seen) == self.K:
                        break
        for e in ENGS:
            self.pending[e] = list(last)

    def emit(self):
        nc = self.nc
        K = self.K
        from contextlib import ExitStack
        with ExitStack() as st:
            esem = {e: st.enter_context(nc.semaphore("s_" + e)) for e in ENGS}
            rsem = {e: [st.enter_context(nc.semaphore("r_%s%d" % (e, i))) for i in range(K)]
                    for e in ENGS if self.ndma[e] > 0}
            for e in ENGS:
                known = {}
                for o in self.ops[e]:
                    need = {}
                    for d in o.deps:
                        if d is o:
                            continue
                        if d.dma:
                            key = ("d", d.eng, d.qidx % K)
                            val = d.qidx // K + 1
                        else:
                            if d.eng == e and e == "pe":
                                continue
                            key = ("c", d.eng)
                            val = d.pos
                        if key not in need or need[key][0] < val:
                            need[key] = (val, d)
                    if o.dma and o.qidx >= K:
                        key = ("d", e, o.qidx % K)
                        val = o.qidx // K
                        if key not in need or need[key][0] < val:
                            need[key] = (val, None)
                    for key, (val, d) in need.items():
                        if known.get(key, -1) >= val:
                            continue
                        known[key] = val
                        if key[0] == "c":
                            d.needs_inc = True
                            o.waits.append(("c", d))
                        else:
                            o.waits.append(("d", key[1], key[2], val))
            for e in ENGS:
                cnt = 0
                for o in self.ops[e]:
                    if (not o.dma) and o.needs_inc:
                        cnt += 1
                        o.sig = cnt
            final = []
            for e in rsem:
                n = self.ndma[e]
                for s in range(K):
                    c = (n - s + K - 1) // K if n > s else 0
                    if c > 0:
                        final.append((rsem[e][s], 16 * c))

            def run(e, eng):
                for o in self.ops[e]:
                    for w in o.waits:
                        if w[0] == "c":
                            eng.wait_ge(esem[w[1].eng], w[1].sig)
                        else:
                            eng.wait_ge(rsem[w[1]][w[2]], 16 * w[3])
                    ins = o.fn(eng)
                    if o.dma:
                        ins.then_inc(rsem[e][o.qidx % K], 16)
                    elif o.needs_inc:
                        ins.then_inc(esem[e], 1)
                if e == "sp":
                    for sem, v in final:
                        eng.wait_ge(sem, v)

            with nc.Block() as block:
                @block.tensor
                def _(pe):
                    run("pe", pe)

                @block.scalar
                def _(act):
                    run("act", act)

                @block.vector
                def _(dve):
                    run("dve", dve)

                @block.gpsimd
                def _(pool):
                    run("pool", pool)

                @block.sync
                def _(sp):
                    run("sp", sp)


class T:
    __slots__ = ("ap", "res")

    def __init__(self, ap, name="", excl=False):
        self.ap = ap
        self.res = Res(name, excl)

    def __getitem__(self, k):
        return self.ap[k]


def build(layers=(0, 1), seqs=(0, 1), phases="ABCDE", dbg=False):
    nc = bass.Bass("TRN2", target_bir_lowering=False)
    p = Prog(nc)
    kind_dbg = "ExternalOutput" if dbg else "Internal"

    def din(name, shape, dt=F32):
        return nc.dram_tensor(name, list(shape), dt, kind="ExternalInput").ap()

    x_d = din("x", [NSEQ, S, D])
    normg_d = din("norm_g", [DEPTH, D])
    win_d = din("w_in", [DEPTH, D, DIN])
    cdiag_d = din("cdiag", [DEPTH, 128, 8, 4, 128])
    cb_d = din("cb", [DEPTH, 128, 8])
    wq_d = din("wq_bd", [DEPTH, 128, 8, 128])
    wk_d = din("wk_bd", [DEPTH, 128, 8, 128])
    wv_d = din("wv_bd", [DEPTH, 128, 8, 128])
    wif_d = din("w_if", [DEPTH, 128, 24, 32])
    bi_d = din("b_i", [DEPTH, 128, 4])
    bf_d = din("b_f", [DEPTH, 128, 4])
    hng_d = din("hn_g", [DEPTH, D])
    wout_d = din("w_out", [DEPTH, 2 * D, D])
    fing_d = din("final_g", [D])
    ident_d = din("ident", [128, 128], BF16)
    tri_d = din("tri", [128, 128])
    amask_d = din("amask", [128, 48, 128], BF16)
    y_d = nc.dram_tensor("y", [NSEQ, S, D], F32, kind="ExternalOutput").ap()

    def dscr(name, shape, dt=BF16):
        return nc.dram_tensor(name, list(shape), dt, kind=kind_dbg).ap()

    qT_d = dscr("qT_s", [NSEQ, 8, 128, S])
    kT_d = dscr("kT_s", [NSEQ, 8, 128, S])
    szT_d = dscr("szT_s", [NSEQ, 8, 128, S])
    v3_d = dscr("v3_s", [NSEQ, 3, 8, 128, 32, 128])
    xmT_d = dscr("xmT_s", [NSEQ, 8, 128, 8 + S])
    som_d = dscr("som_s", [NSEQ, S, D])
    szm_d = dscr("szm_s", [NSEQ, S, D])
    mixT_d = dscr("mixT_s", [NSEQ, 16, 128, S])
    qmT_d = dscr("qmT_s", [NSEQ, 8, 128, S])
    kmT_d = dscr("kmT_s", [NSEQ, 8, 128, S])
    km_d = dscr("km_s", [NSEQ, S, D])
    vm_d = dscr("vm_s", [NSEQ, S, D])
    x1_d = dscr("x1_s", [NSEQ, S, D], F32)
    gp_dbg = dscr("gp_s", [NSEQ, 128, 4, 128], F32) if dbg else None

    dres = {}

    def DR(*key):
        if key not in dres:
            dres[key] = Res(str(key))
        return dres[key]

    def persist(name, shape, dt):
        return T(nc.alloc_sbuf_tensor(name, list(shape), dt), name)

    ident = persist("ident_sb", [128, 128], BF16)
    tri = persist("tri_sb", [128, 128], F32)
    ones_f = persist("ones_f", [128, 128], F32)
    ones_b = persist("ones_b", [128, 128], BF16)
    tri_b = persist("tri_b", [128, 128], BF16)
    amask = persist("amask_sb", [128, 48, 128], BF16)
    cst = persist("cst", [128, 8], F32)
    gp = persist("gp", [128, 4, 128], F32)
    ARENA_W = 47 * 1024
    arena = nc.alloc_sbuf_tensor("arena", [128, ARENA_W], F32)
    banks = [nc.alloc_psum_tensor("bank%d" % i, [128, 512], F32) for i in range(8)]

    class Arena:
        def __init__(self):
            self.off = 0

        def get(self, name, shape, dt):
            n = 1
            for s_ in shape[1:]:
                n *= s_
            nbytes = n * (4 if dt == F32 else 2)
            nbytes = (nbytes + 63) // 64 * 64
            v = arena[:, self.off // 4:(self.off + nbytes) // 4]
            if dt != F32:
                v = v.bitcast(dt)
            v = v[:, 0:n]
            if len(shape) == 3:
                v = v.rearrange("p (a b) -> p a b", a=shape[1])
            elif len(shape) == 4:
                v = v.rearrange("p (a b c) -> p a b c", a=shape[1], b=shape[2])
            self.off += nbytes
            assert self.off <= ARENA_W * 4, (name, self.off)
            return T(v, name)

    def psum_views(dt=F32):
        out = []
        for b_ in banks:
            v = b_[:, :]
            if dt != F32:
                v = v.bitcast(dt)
            out.append(T(v, "bank", True))
        return out

    def dma(q, out, in_, reads=(), writes=()):
        p.add(q, lambda e: e.dma_start(out=out, in_=in_), reads=reads, writes=writes, dma=True)

    def mm(out, lhsT, rhs, start, stop, reads, writes, skip=False):
        if skip:
            p.add("pe", lambda e: e.matmul(out, lhsT=lhsT, rhs=rhs, start=start, stop=stop,
                                           skip_group_check=True), reads=reads, writes=writes)
        else:
            p.add("pe", lambda e: e.matmul(out, lhsT=lhsT, rhs=rhs, start=start, stop=stop),
                  reads=reads, writes=writes)

    def act(out, in_, func, reads, writes, bias=None, scale=1.0, accum=None):
        kw = {}
        if bias is not None:
            kw["bias"] = bias
        if accum is not None:
            kw["accum_out"] = accum
        p.add("act", lambda e: e.activation(out=out, in_=in_, func=func, scale=scale, **kw),
              reads=reads, writes=writes)

    def dve(fn, reads, writes):
        p.add("dve", fn, reads=reads, writes=writes)

    dma("sp", ident[:], ident_d, writes=[ident.res])
    dma("sp", tri[:], tri_d, writes=[tri.res])
    dma("sp", amask[:], amask_d, writes=[amask.res])
    dve(lambda e: e.memset(ones_f[:], 1.0), [], [ones_f.res])
    dve(lambda e: e.memset(ones_b[:], 1.0), [], [ones_b.res])
    dve(lambda e: e.tensor_copy(out=tri_b[:], in_=tri[:]), [tri.res], [tri_b.res])
    dve(lambda e: e.memset(cst[:, 0:1], EPS), [], [cst.res])
    dve(lambda e: e.memset(cst[:, 1:2], -math.log(16.0)), [], [cst.res])
    dve(lambda e: e.memset(cst[:, 2:3], 1.0), [], [cst.res])

    zpad = persist("zpad", [128, 8, 8], BF16)
    dve(lambda e: e.memset(zpad[:], 0.0), [], [zpad.res])
    for b_ in range(NSEQ):
        dma("sp", xmT_d[b_, :, :, 0:8].rearrange("c p t -> p c t"), zpad[:], reads=[zpad.res], writes=[DR("xmTpad", b_)])

    evac_rr = [0]

    def copy_evac(out, in_, reads, writes, scale=None):
        evac_rr[0] ^= 1
        if evac_rr[0]:
            act(out, in_, AF.Copy, reads, writes, scale=(1.0 if scale is None else scale))
        else:
            if scale is None:
                dve(lambda e: e.tensor_copy(out=out, in_=in_), reads, writes)
            else:
                dve(lambda e: e.tensor_scalar_mul(out=out, in0=in_, scalar1=scale), reads, writes)

    def phase_A(L, b):
        ar = Arena()
        hT = ar.get("hT", [128, 8, S], BF16)
        hres = [Res("hT%d" % i) for i in range(8)]
        xin = [ar.get("xin%d" % i, [128, 4, D], F32) for i in range(2)]
        xn = [ar.get("xn%d" % i, [128, D], BF16) for i in range(2)]
        junk = ar.get("junk", [128, D], BF16)
        W = [ar.get("W%d" % i, [128, 8, 512], BF16) for i in range(2)]
        ofm = [ar.get("ofm%d" % i, [128, S], BF16) for i in range(2)]
        otm = [ar.get("otm%d" % i, [128, 4, 512], BF16) for i in range(2)]
        vst = [ar.get("vst%d" % i, [128, 4, 32, 128], BF16) for i in range(1)]
        gn = ar.get("gn", [128, D], F32)
        stt = ar.get("stt", [128, 3, 32], F32)
        ps = psum_views()
        psT = [T(banks[i][:, :].bitcast(BF16), "psT", True) for i in range(2)]

        src = x_d if L == 0 else x1_d
        dma("sp", gn[:], normg_d[L].partition_broadcast(128), writes=[gn.res])

        def load_x(tq):
            dma("sp", xin[tq % 2][:], src[b, tq * 512:(tq + 1) * 512, :].rearrange("(n p) c -> p n c", p=128),
                reads=[DR("x1", b, tq)] if L > 0 else [], writes=[xin[tq % 2].res])

        jobs = []
        for wb in (0, 1):
            jobs.append(("fm", wb, AF.Copy, None))
        for wb in (2, 3):
            jobs.append(("fm", wb, AF.Copy, None))
        for wb in (8, 9):
            jobs.append(("fm", wb, AF.Copy, None))
        for di in range(3):
            for wb in (4, 5):
                jobs.append(("v", wb, AF.Copy, di))
        for wb in (6, 7):
            jobs.append(("fm", wb, AF.Silu, None))
        for wb in (12, 13):
            jobs.append(("tm", wb, AF.Silu, None))
        for wb in (10, 11):
            jobs.append(("tm", wb, AF.Sigmoid, None))
        wl = []
        for jb in jobs:
            if not wl or wl[-1] != jb[1]:
                wl.append(jb[1])
        wl_state = {"next": 0}

        def load_w():
            i = wl_state["next"]
            if i >= len(wl):
                return
            wb = wl[i]
            dma("pool", W[i % 2][:], win_d[L, :, wb * 512:(wb + 1) * 512].rearrange("(kc p) j -> p kc j", p=128),
                writes=[W[i % 2].res])
            wl_state["next"] = i + 1

        load_x(0)
        load_w()
        for tq in range(8):
            if tq + 1 < 8:
                load_x(tq + 1)
            xi = xin[tq % 2]
            for n in range(4):
                ti = tq * 4 + n
                xb = xn[ti % 2]
                act(junk[:], xi[:, n, :], AF.Square, [xi.res], [junk.res, stt.res], accum=stt[:, 0, ti:ti + 1])
                act(stt[:, 1, ti:ti + 1], stt[:, 0, ti:ti + 1], AF.Sqrt, [stt.res, cst.res], [stt.res],
                    bias=cst[:, 0:1], scale=1.0 / D)
                dve(lambda e, ti=ti: e.reciprocal(out=stt[:, 2, ti:ti + 1], in_=stt[:, 1, ti:ti + 1]),
                    [stt.res], [stt.res])
                dve(lambda e, xi=xi, n=n, ti=ti, xb=xb: e.scalar_tensor_tensor(
                    out=xb[:], in0=xi[:, n, :], scalar=stt[:, 2, ti:ti + 1], in1=gn[:],
                    op0=ALU.mult, op1=ALU.mult), [xi.res, stt.res, gn.res], [xb.res])
                pt = psT[ti % 2]
                for kc in range(8):
                    p.add("pe", lambda e, pt=pt, xb=xb, kc=kc: e.transpose(
                        pt[:, kc * 128:(kc + 1) * 128], xb[:, kc * 128:(kc + 1) * 128], ident[:]),
                        reads=[xb.res, ident.res], writes=[pt.res])
                copy_evac(hT[:, :, ti * 128:(ti + 1) * 128],
                          pt[:, :].rearrange("p (k t) -> p k t", k=8), [pt.res], [hres[tq]])
        pb = [2]

        def next_bank():
            r = ps[pb[0]]
            pb[0] = pb[0] + 1 if pb[0] < 7 else 2
            return r

        fm_i = [0]
        tm_i = [0]
        wcur = -1
        wprev = None
        for (kind, wb, func, di) in jobs:
            if wprev != wb:
                wcur += 1
                wprev = wb
                load_w()
            Wt = W[wcur % 2]
            if kind == "fm":
                for g in range(4):
                    col = wb * 512 + g * 128
                    if col < 1024:
                        dst, scale = qT_d[b, col // 128], 128 ** -0.5
                        dr = DR("qT", b, col // 128)
                    elif col < 2048:
                        dst, scale = kT_d[b, (col - 1024) // 128], None
                        dr = DR("kT", b, (col - 1024) // 128)
                    elif col < 4096:
                        dst, scale = szT_d[b, (col - 3072) // 128], None
                        dr = DR("szT", b, (col - 3072) // 128)
                    else:
                        dst, scale = xmT_d[b, (col - 4096) // 128][:, 8:8 + S], None
                        dr = DR("xmT", b, (col - 4096) // 128)
                    ob = ofm[fm_i[0] % 2]
                    fm_i[0] += 1
                    for tb in range(8):
                        bk = next_bank()
                        for kc in range(8):
                            mm(bk[:, :], Wt[:, kc, g * 128:(g + 1) * 128], hT[:, kc, tb * 512:(tb + 1) * 512],
                               kc == 0, kc == 7, [Wt.res, hres[tb]], [bk.res])
                        if func == AF.Copy:
                            copy_evac(ob[:, tb * 512:(tb + 1) * 512], bk[:, :], [bk.res], [ob.res], scale=scale)
                        else:
                            act(ob[:, tb * 512:(tb + 1) * 512], bk[:, :], func, [bk.res], [ob.res])
                    dma("sp", dst, ob[:], reads=[ob.res], writes=[dr])
            elif kind == "tm":
                dst_t, nm = (szm_d, "szm") if wb >= 12 else (som_d, "som")
                c0 = (wb % 2) * 512
                for ti in range(32):
                    ob = otm[tm_i[0] % 2]
                    bk = next_bank()
                    for kc in range(8):
                        mm(bk[:, :], hT[:, kc, ti * 128:(ti + 1) * 128], Wt[:, kc, :], kc == 0, kc == 7,
                           [Wt.res, hres[ti // 4]], [bk.res])
                    act(ob[:, ti % 4, :], bk[:, :], func, [bk.res], [ob.res])
                    if ti % 4 == 3:
                        tq = ti // 4
                        dma("sp", dst_t[b, tq * 512:(tq + 1) * 512, c0:c0 + 512].rearrange("(n p) c -> p n c", p=128),
                            ob[:], reads=[ob.res], writes=[DR(nm, b, tq)])
                        tm_i[0] += 1
            else:
                d_ = DILS[di]
                hh0 = (wb % 2) * 4
                vs = vst[0]
                for blk in range(32):
                    nblk = 32 // d_
                    r, n = blk // nblk, blk % nblk
                    base = r + d_ * 128 * n
                    bk = next_bank()
                    for kc in range(8):
                        mm(bk[:, :], hT[:, kc, base:base + d_ * 127 + 1:d_], Wt[:, kc, :], kc == 0, kc == 7,
                           [Wt.res] + [hres[i] for i in range(base // 512, min(8, (base + d_ * 128 - 1) // 512 + 1))],
                           [bk.res])
                    copy_evac(vs[:, :, blk, :], bk[:, :].rearrange("p (h c) -> p h c", h=4), [bk.res], [vs.res])
                for hh in range(4):
                    dma("sp", v3_d[b, di, hh0 + hh], vs[:, hh, :, :], reads=[vs.res], writes=[DR("v3", b, hh0 + hh)])

    def phase_B(L, b):
        ar = Arena()
        qT = [ar.get("qT%d" % i, [128, S], BF16) for i in range(2)]
        kT = [ar.get("kT%d" % i, [128, S], BF16) for i in range(2)]
        szT = [ar.get("szT%d" % i, [128, S], BF16) for i in range(2)]
        v3 = [[ar.get("v%d_%d" % (di, i), [128, 32, 128], BF16) for di in range(3)] for i in range(2)]
        accn = ar.get("accn", [128, S], F32)
        accd = ar.get("accd", [128, S], F32)
        att = [ar.get("att%d" % i, [128, S], BF16) for i in range(2)]
        PT = [ar.get("PT%d" % i, [128, 256], BF16) for i in range(4)]
        t1 = [ar.get("t1_%d" % i, [128, 512], F32) for i in range(2)]
        t2 = [ar.get("t2_%d" % i, [128, 512], F32) for i in range(2)]
        ps = psum_views()
        sc_i = [0]
        acc_i = [0]

        def load_head(h):
            i = h % 2
            dma("sp", qT[i][:], qT_d[b, h], reads=[DR("qT", b, h)], writes=[qT[i].res])
            dma("sp", kT[i][:], kT_d[b, h], reads=[DR("kT", b, h)], writes=[kT[i].res])
            dma("sp", szT[i][:], szT_d[b, h], reads=[DR("szT", b, h)], writes=[szT[i].res])
            for di in range(3):
                dma("sp", v3[i][di][:], v3_d[b, di, h], reads=[DR("v3", b, h)], writes=[v3[i][di].res])

        pend = []
        LAG = 2

        def drain(keep):
            while sum(1 for k_, _f in pend if k_ == "pv") > keep:
                pend.pop(0)[1]()
            while pend and pend[0][0] == "ev":
                pend.pop(0)[1]()

        def qblock(h, di, qsl, ksl_prev, ksl_cur, blk_prev, blk_cur, outn, outd, first, last):
            i = h % 2
            sb = ps[sc_i[0] % 4]
            pt = PT[sc_i[0] % 4]
            sc_i[0] += 1
            mi = (h * 3 + di) * 2
            c0 = 0 if ksl_prev is not None else 128
            if ksl_prev is not None:
                mm(sb[:, 0:128], kT[i][:, ksl_prev], qT[i][:, qsl], True, False, [kT[i].res, qT[i].res], [sb.res])
                mm(sb[:, 0:128], ident[:], amask[:, mi + 1, :], False, True, [ident.res, amask.res], [sb.res])
            mm(sb[:, 128:256], kT[i][:, ksl_cur], qT[i][:, qsl], True, False, [kT[i].res, qT[i].res], [sb.res])
            mm(sb[:, 128:256], ident[:], amask[:, mi, :], False, True, [ident.res, amask.res], [sb.res])
            act(pt[:, c0:256], sb[:, c0:256], AF.Exp, [sb.res], [pt.res])
            vv = v3[i][di]

            def part2():
                for (o_, lw) in ((outn, None), (outd, ones_b)):
                    seq = []
                    if ksl_prev is not None:
                        seq.append((blk_prev, pt[:, 0:128]))
                    seq.append((blk_cur, pt[:, 128:256]))
                    for si, (bk_, rhs) in enumerate(seq):
                        lhs = vv[:, bk_, :] if lw is None else lw[:]
                        st_ = first and si == 0
                        sp_ = last and si == len(seq) - 1
                        mm(o_[0], lhs, rhs, st_, sp_, [vv.res if lw is None else lw.res, pt.res], [o_[1]], skip=True)

            pend.append(("pv", part2))
            drain(LAG)

        def evac16(bn, bd, n, rq):
            def f():
                for (bk_, acc, eng) in ((bn, accn, "act"), (bd, accd, "dve")):
                    dst = acc[:, n * 2048:(n + 1) * 2048].rearrange("p (j r) -> p j r", r=16)[:, :, rq * 4:rq * 4 + 4]
                    dst = dst.rearrange("p j r -> p r j")
                    srcv = bk_[:, :].rearrange("p (r j) -> p r j", r=4)
                    if eng == "act":
                        act(dst, srcv, AF.Copy, [bk_.res], [acc.res])
                    else:
                        dve(lambda e, dst=dst, srcv=srcv: e.tensor_copy(out=dst, in_=srcv), [bk_.res], [acc.res])
            return f

        def combine(bn, bd, tb, i):
            def f():
                a1, a2 = t1[tb % 2], t2[tb % 2]
                cs = slice(tb * 512, (tb + 1) * 512)
                dve(lambda e: e.tensor_tensor(out=a1[:], in0=bn[:, :], in1=accn[:, cs], op=ALU.add),
                    [bn.res, accn.res], [a1.res])
                dve(lambda e: e.tensor_tensor(out=a2[:], in0=bd[:, :], in1=accd[:, cs], op=ALU.add),
                    [bd.res, accd.res], [a2.res])
                dve(lambda e: e.reciprocal(out=a2[:], in_=a2[:]), [a2.res], [a2.res])
                dve(lambda e: e.tensor_tensor(out=a1[:], in0=a1[:], in1=a2[:], op=ALU.mult),
                    [a1.res, a2.res], [a1.res])
                dve(lambda e: e.tensor_tensor(out=att[i][:, cs], in0=a1[:], in1=szT[i][:, cs], op=ALU.mult),
                    [a1.res, szT[i].res], [att[i].res])
            return f

        load_head(0)
        for h in range(8):
            if h + 1 < 8:
                load_head(h + 1)
            i = h % 2
            for n in range(2):
                for rq in range(4):
                    bn = ps[4 + acc_i[0] % 2]
                    bd = ps[6 + acc_i[0] % 2]
                    acc_i[0] += 1
                    for rr in range(4):
                        r = rq * 4 + rr
                        qsl = slice(n * 2048 + r, n * 2048 + r + 16 * 127 + 1, 16)
                        kprev = slice((n - 1) * 2048 + r, (n - 1) * 2048 + r + 16 * 127 + 1, 16) if n >= 1 else None
                        qblock(h, 2, qsl, kprev, qsl, r * 2 + n - 1, r * 2 + n,
                               (bn[:, rr * 128:(rr + 1) * 128], bn.res), (bd[:, rr * 128:(rr + 1) * 128], bd.res),
                               True, True)
                    pend.append(("ev", evac16(bn, bd, n, rq)))
            for tb in range(8):
                bn = ps[4 + acc_i[0] % 2]
                bd = ps[6 + acc_i[0] % 2]
                acc_i[0] += 1
                for qb in range(4):
                    n = tb * 4 + qb
                    qsl = slice(n * 128, (n + 1) * 128)
                    kprev = slice((n - 1) * 128, n * 128) if n >= 1 else None
                    qblock(h, 0, qsl, kprev, qsl, n - 1, n,
                           (bn[:, qb * 128:(qb + 1) * 128], bn.res), (bd[:, qb * 128:(qb + 1) * 128], bd.res),
                           qb == 0, False)
                for r in range(4):
                    qsl = slice(tb * 512 + r, tb * 512 + r + 4 * 127 + 1, 4)
                    kprev = slice((tb - 1) * 512 + r, (tb - 1) * 512 + r + 4 * 127 + 1, 4) if tb >= 1 else None
                    qblock(h, 1, qsl, kprev, qsl, r * 8 + tb - 1, r * 8 + tb,
                           (bn[:, r:r + 4 * 127 + 1:4], bn.res), (bd[:, r:r + 4 * 127 + 1:4], bd.res), False, r == 3)
                pend.append(("ev", combine(bn, bd, tb, i)))
            drain(0)
            dma("sp", mixT_d[b, h], att[i][:], reads=[att[i].res], writes=[DR("mixT", b, h)])

    def phase_C(L, b):
        ar = Arena()
        xf = [ar.get("xf%d" % i, [128, 8, 520], BF16) for i in range(2)]
        xc = [ar.get("xc%d" % i, [128, 8, 512], BF16) for i in range(2)]
        qs = [ar.get("qs%d" % i, [128, 8, 512], BF16) for i in range(2)]
        ks = [ar.get("ks%d" % i, [128, 8, 512], BF16) for i in range(2)]
        vs = [ar.get("vs%d" % i, [128, 8, 512], BF16) for i in range(2)]
        ktm = [ar.get("ktm%d" % i, [128, 4, D], BF16) for i in range(2)]
        vtm = [ar.get("vtm%d" % i, [128, 4, D], BF16) for i in range(2)]
        cdg = ar.get("cdg", [128, 8, 4, 128], BF16)
        wq = ar.get("wq", [128, 8, 128], BF16)
        wk = ar.get("wk", [128, 8, 128], BF16)
        wv = ar.get("wv", [128, 8, 128], BF16)
        wif = ar.get("wif", [128, 24, 32], BF16)
        cb = ar.get("cb", [128, 8], F32)
        bib = ar.get("bib", [128, 4], F32)
        bfb = ar.get("bfb", [128, 4], F32)
        graw = ar.get("graw", [128, 32, 8], F32)
        gt = ar.get("gt", [128, 4, 128], F32)
        xh = ar.get("xh", [128, 3, 128], BF16)
        xr_ = ar.get("xr_", [128, 2, 128], F32)
        ps = psum_views()

        for c_ in range(8):
            dma("pool", cdg[:, c_, :, :], cdiag_d[L, :, c_, :, :], writes=[cdg.res])
        dma("pool", wq[:], wq_d[L], writes=[wq.res])
        dma("pool", wk[:], wk_d[L], writes=[wk.res])
        dma("pool", wv[:], wv_d[L], writes=[wv.res])
        dma("pool", wif[:], wif_d[L], writes=[wif.res])
        dma("sp", cb[:], cb_d[L], writes=[cb.res])
        dma("sp", bib[:], bi_d[L], writes=[bib.res])
        dma("sp", bfb[:], bf_d[L], writes=[bfb.res])
        dve(lambda e: e.tensor_scalar_mul(out=bfb[:], in0=bfb[:], scalar1=-1.0), [bfb.res], [bfb.res])

        def load(tb):
            i = tb % 2
            dma("sp", xf[i][:], xmT_d[b, :, :, tb * 512:tb * 512 + 520].rearrange("c p t -> p c t"),
                reads=[DR("xmT", b, c) for c in range(8)] + [DR("xmTpad", b)], writes=[xf[i].res])

        load(0)
        cv_i = [0]
        LV = int(os.environ.get("CBIS", "9"))
        for tb in range(8):
            i = tb % 2
            if tb + 1 < 8:
                load(tb + 1)
            for c in range(8 if LV >= 2 else 0):
                bk = ps[cv_i[0] % 2]
                cv_i[0] += 1
                for w in range(4):
                    mm(bk[:, :], cdg[:, c, w, :], xf[i][:, c, 5 + w:5 + w + 512], w == 0, w == 3, [cdg.res, xf[i].res], [bk.res])
                act(xc[i][:, c, :], bk[:, :], AF.Silu, [bk.res, cb.res], [xc[i].res], bias=cb[:, c:c + 1])
                if LV < 3:
                    continue
                for (wt, srcv, sres, dstt, bi_) in ((wq, xc[i][:, c, :], xc[i].res, qs[i], 2),
                                                   (wk, xc[i][:, c, :], xc[i].res, ks[i], 3),
                                                   (wv, xf[i][:, c, 8:520], xf[i].res, vs[i], 2)):
                    bk2 = ps[bi_] if wt is not wv else ps[2 + (c % 2)]
                    mm(bk2[:, :], wt[:, c, :], srcv, True, True, [wt.res, sres], [bk2.res])
                    copy_evac(dstt[:, c, :], bk2[:, :], [bk2.res], [dstt.res])
                if LV < 4:
                    continue
                for (wt, lsrc, off, dstt, bi_) in ((wk, xc[i], 0, ktm[i], 4), (wv, xf[i], 8, vtm[i], 5)):
                    bk3 = ps[bi_]
                    for ti in range(4):
                        mm(bk3[:, ti * 128:(ti + 1) * 128], lsrc[:, c, off + ti * 128:off + (ti + 1) * 128], wt[:, c, :],
                           True, True, [wt.res, lsrc.res], [bk3.res])
                    copy_evac(dstt[:, :, c * 128:(c + 1) * 128], bk3[:, :].rearrange("p (t c) -> p t c", t=4),
                              [bk3.res], [dstt.res])
            bg = ps[6]
            for ti in range(4 if LV >= 5 else 0):
                k_ = 0
                for (srct, w0) in ((qs[i], 0), (ks[i], 8), (vs[i], 16)):
                    for c in range(8):
                        mm(bg[:, ti * 32:(ti + 1) * 32], srct[:, c, ti * 128:(ti + 1) * 128], wif[:, w0 + c, :],
                           k_ == 0, k_ == 23, [srct.res, wif.res], [bg.res])
                        k_ += 1
            dve(lambda e, tb=tb, bg=bg: e.tensor_copy(out=graw[:, tb * 4:(tb + 1) * 4, :],
                                                      in_=bg[:, 0:128].rearrange("p (t g) -> p t g", t=4)[:, :, 0:8]),
                [bg.res], [graw.res])
            dma("sp", qmT_d[b, :, :, tb * 512:(tb + 1) * 512].rearrange("c p t -> p c t"), qs[i][:],
                reads=[qs[i].res], writes=[DR("qmT", b, tb)])
            dma("sp", kmT_d[b, :, :, tb * 512:(tb + 1) * 512].rearrange("c p t -> p c t"), ks[i][:],
                reads=[ks[i].res], writes=[DR("kmT", b, tb)])
            dma("sp", km_d[b, tb * 512:(tb + 1) * 512, :].rearrange("(n p) c -> p n c", p=128), ktm[i][:],
                reads=[ktm[i].res], writes=[DR("km", b, tb)])
            dma("sp", vm_d[b, tb * 512:(tb + 1) * 512, :].rearrange("(n p) c -> p n c", p=128), vtm[i][:],
                reads=[vtm[i].res], writes=[DR("vm", b, tb)])
        if LV < 6:
            return
        li = gt[:, 0, :].rearrange("p (j h) -> p j h", h=4)
        ef = gt[:, 1, :].rearrange("p (j h) -> p j h", h=4)
        for hd in range(4):
            dve(lambda e, hd=hd: e.tensor_scalar_add(out=li[:, :, hd], in0=graw[:, :, hd], scalar1=bib[:, hd:hd + 1]),
                [graw.res, bib.res], [gt.res])
            act(ef[:, :, hd], graw[:, :, 4 + hd], AF.Exp, [graw.res, bfb.res], [gt.res], bias=bfb[:, hd:hd + 1], scale=-1.0)
        if LV < 7:
            return
        act(gt[:, 1, :], gt[:, 1, :], AF.Ln, [gt.res, cst.res], [gt.res], bias=cst[:, 2:3], scale=1.0)
        if LV < 8:
            return
        bc = ps[7]
        dve(lambda e: e.tensor_copy(out=xh[:, 0, :], in_=gt[:, 1, :]), [gt.res], [xh.res])
        dve(lambda e: e.tensor_tensor(out=xr_[:, 0, :], in0=gt[:, 1, :], in1=xh[:, 0, :], op=ALU.subtract),
            [gt.res, xh.res], [xr_.res])
        dve(lambda e: e.tensor_copy(out=xh[:, 1, :], in_=xr_[:, 0, :]), [xr_.res], [xh.res])
        dve(lambda e: e.tensor_tensor(out=xr_[:, 1, :], in0=xr_[:, 0, :], in1=xh[:, 1, :], op=ALU.subtract),
            [xr_.res, xh.res], [xr_.res])
        dve(lambda e: e.tensor_copy(out=xh[:, 2, :], in_=xr_[:, 1, :]), [xr_.res], [xh.res])
        for k_ in range(3):
            mm(bc[:, 0:128], tri_b[:], xh[:, k_, :], k_ == 0, k_ == 2, [tri_b.res, xh.res], [bc.res])
        for k_ in range(3):
            mm(bc[:, 128:256], ones_b[:], xh[:, k_, :], k_ == 0, k_ == 2, [ones_b.res, xh.res], [bc.res])
        if LV == 8:
            return
        act(gp[:, 0, :], bc[:, 0:128], AF.Exp, [bc.res, cst.res], [gp.res], bias=cst[:, 1:2], scale=-1.0)
        if LV == 10:
            return
        dve(lambda e: e.tensor_tensor(out=gt[:, 2, :], in0=bc[:, 0:128], in1=gt[:, 0, :], op=ALU.add),
            [bc.res, gt.res], [gt.res])
        act(gp[:, 1, :], gt[:, 2, :], AF.Exp, [gt.res], [gp.res])
        act(gp[:, 3, :], bc[:, 128:256], AF.Exp, [bc.res], [gp.res], scale=-1.0)
        if LV == 12:
            return
        dve(lambda e: e.tensor_tensor(out=gp[:, 2, :], in0=gp[:, 1, :], in1=gp[:, 3, :], op=ALU.mult),
            [gp.res], [gp.res])
        if LV == 13:
            return
        if dbg:
            dma("sp", gp_dbg[b], gp[:], reads=[gp.res])

    def phase_D(L, b):
        ar = Arena()
        qb_ = [ar.get("qb%d" % i, [128, 8, 512], BF16) for i in range(2)]
        kb_ = [ar.get("kb%d" % i, [128, 8, 512], BF16) for i in range(2)]
        ktm = [ar.get("ktm%d" % i, [128, 4, D], BF16) for i in range(2)]
        vtm = [ar.get("vtm%d" % i, [128, 4, D], BF16) for i in range(2)]
        som = [ar.get("som%d" % i, [128, 4, D], BF16) for i in range(2)]
        szm = [ar.get("szm%d" % i, [128, 4, D], BF16) for i in range(2)]
        C32 = ar.get("C32", [128, 4, 2, 260], F32)
        Cbf = ar.get("Cbf", [128, 4, 2, 260], BF16)
        Cres = [Res("C%d" % i) for i in range(4)]
        Cbres = [Res("Cb%d" % i) for i in range(4)]
        St = [ar.get("St%d" % i, [128, 128], BF16) for i in range(2)]
        vp = [ar.get("vp%d" % i, [128, 4, 260], BF16) for i in range(2)]
        vpp = [ar.get("vpp%d" % i, [128, 4, 260], BF16) for i in range(2)]
        hm = [ar.get("hm%d" % i, [128, D], F32) for i in range(2)]
        sq = ar.get("sq", [128, D], F32)
        gz = ar.get("gz", [128, D], F32)
        mls = [ar.get("mls%d" % i, [128, D], BF16) for i in range(2)]
        hng = ar.get("hng", [128, D], F32)
        mxs = [ar.get("mxs%d" % i, [128, 8, 512], BF16) for i in range(2)]
        sm = [ar.get("sm%d" % i, [128, 8, 4], F32) for i in range(2)]
        ds_ = [ar.get("ds%d" % i, [128, 4], F32) for i in range(4)]
        ps = psum_views()
        psTb = [T(banks[6 + i][:, :].bitcast(BF16), "psTb", True) for i in range(2)]

        dma("sp", hng[:], hng_d[L].partition_broadcast(128), writes=[hng.res])
        dve(lambda e: e.memset(C32[:, :, :, :], 0.0), [], Cres)
        dve(lambda e: e.memset(Cbf[:, :, :, :], 0.0), [], Cbres)

        def load(tb):
            i = tb % 2
            cs = slice(tb * 512, (tb + 1) * 512)
            dma("sp", qb_[i][:], qmT_d[b, :, :, cs].rearrange("c p t -> p c t"), reads=[DR("qmT", b, tb)], writes=[qb_[i].res])
            dma("sp", kb_[i][:], kmT_d[b, :, :, cs].rearrange("c p t -> p c t"), reads=[DR("kmT", b, tb)], writes=[kb_[i].res])
            for (t_, d_, nm) in ((ktm, km_d, "km"), (vtm, vm_d, "vm"), (som, som_d, "som"), (szm, szm_d, "szm")):
                dma("sp", t_[i][:], d_[b, cs, :].rearrange("(n p) c -> p n c", p=128), reads=[DR(nm, b, tb)], writes=[t_[i].res])

        load(0)
        cnt = [0]
        for tb in range(8):
            i = tb % 2
            if tb + 1 < 8:
                load(tb + 1)
            mx = mxs[i]
            for jj in range(4):
                j = tb * 4 + jj
                tk = slice(jj * 128, (jj + 1) * 128)
                vpi, vppi = vp[j % 2], vpp[j % 2]
                for hd in range(4):
                    g_ = j * 4 + hd
                    act(vpi[:, hd, 0:256], vtm[i][:, jj, hd * 256:(hd + 1) * 256], AF.Copy, [vtm[i].res, gp.res], [vpi.res],
                        scale=gp[:, 1, g_:g_ + 1])
                    act(vppi[:, hd, 0:256], vtm[i][:, jj, hd * 256:(hd + 1) * 256], AF.Copy, [vtm[i].res, gp.res], [vppi.res],
                        scale=gp[:, 2, g_:g_ + 1])
                p.add("pool", lambda e, vpi=vpi, j=j: e.tensor_copy(out=vpi[:, :, 256], in_=gp[:, 1, j * 4:(j + 1) * 4]),
                      reads=[gp.res], writes=[vpi.res])
                p.add("pool", lambda e, vppi=vppi, j=j: e.tensor_copy(out=vppi[:, :, 256], in_=gp[:, 2, j * 4:(j + 1) * 4]),
                      reads=[gp.res], writes=[vppi.res])
                hmi = hm[j % 2]
                dve(lambda e, i=i, jj=jj: e.tensor_tensor(out=gz[:], in0=hng[:], in1=szm[i][:, jj, :], op=ALU.mult),
                    [hng.res, szm[i].res], [gz.res])
                for hd in range(4):
                    g_ = j * 4 + hd
                    k_ = cnt[0]
                    cnt[0] += 1
                    bs = ps[k_ % 2]
                    bn = ps[2 + k_ % 2]
                    sti = St[k_ % 2]
                    dsi = ds_[k_ % 4]
                    for half in range(2):
                        mm(bs[:, 0:128], kb_[i][:, 2 * hd + half, tk], qb_[i][:, 2 * hd + half, tk], half == 0, half == 1,
                           [kb_[i].res, qb_[i].res], [bs.res])
                    dve(lambda e, sti=sti, bs=bs: e.tensor_tensor(out=sti[:], in0=bs[:, 0:128], in1=tri[:], op=ALU.mult),
                        [bs.res, tri.res], [sti.res])
                    for half in range(2):
                        mm(bn[:, 0:257], qb_[i][:, 2 * hd + half, tk], Cbf[:, hd, half, 0:257], half == 0, False,
                           [qb_[i].res, Cbres[hd]], [bn.res])
                    mm(bn[:, 0:257], sti[:], vpi[:, hd, 0:257], False, True, [sti.res, vpi.res], [bn.res])
                    for half in range(2):
                        bd = ps[4 + half]
                        mm(bd[:, 0:257], ktm[i][:, jj, hd * 256 + half * 128:hd * 256 + (half + 1) * 128], vppi[:, hd, 0:257],
                           True, True, [ktm[i].res, vppi.res], [bd.res])
                        dve(lambda e, hd=hd, half=half, bd=bd, g_=g_: e.scalar_tensor_tensor(
                            out=C32[:, hd, half, 0:257], in0=C32[:, hd, half, 0:257], scalar=gp[:, 3, g_:g_ + 1],
                            in1=bd[:, 0:257], op0=ALU.mult, op1=ALU.add), [Cres[hd], gp.res, bd.res], [Cres[hd]])
                    act(Cbf[:, hd, :, 0:257], C32[:, hd, :, 0:257], AF.Copy, [Cres[hd]], [Cbres[hd]])
                    act(dsi[:, 0:1], bn[:, 256:257], AF.Abs, [bn.res, gp.res], [dsi.res], scale=gp[:, 0, g_:g_ + 1])
                    dve(lambda e, dsi=dsi: e.tensor_scalar_max(out=dsi[:, 1:2], in0=dsi[:, 0:1], scalar1=1.0), [dsi.res], [dsi.res])
                    dve(lambda e, dsi=dsi: e.reciprocal(out=dsi[:, 2:3], in_=dsi[:, 1:2]), [dsi.res], [dsi.res])
                    dve(lambda e, dsi=dsi, g_=g_: e.tensor_tensor(out=dsi[:, 3:4], in0=dsi[:, 2:3], in1=gp[:, 0, g_:g_ + 1], op=ALU.mult),
                        [dsi.res, gp.res], [dsi.res])
                    dve(lambda e, hmi=hmi, hd=hd, bn=bn, dsi=dsi, i=i, jj=jj: e.scalar_tensor_tensor(
                        out=hmi[:, hd * 256:(hd + 1) * 256], in0=bn[:, 0:256], scalar=dsi[:, 3:4],
                        in1=som[i][:, jj, hd * 256:(hd + 1) * 256], op0=ALU.mult, op1=ALU.mult),
                        [bn.res, dsi.res, som[i].res], [hmi.res])
                smi = sm[j % 2]
                hv = hmi[:, :].rearrange("p (h d) -> p h d", h=4)
                dve(lambda e, smi=smi, hv=hv: e.reduce_sum(out=smi[:, 0, :], in_=hv, axis=AX.X), [hmi.res], [smi.res])
                act(sq[:], hmi[:], AF.Square, [hmi.res], [sq.res])
                dve(lambda e, smi=smi: e.reduce_sum(out=smi[:, 1, :], in_=sq[:, :].rearrange("p (h d) -> p h d", h=4), axis=AX.X),
                    [sq.res], [smi.res])
                dve(lambda e, smi=smi: e.tensor_scalar_mul(out=smi[:, 2, :], in0=smi[:, 0, :], scalar1=1.0 / 256), [smi.res], [smi.res])
                dve(lambda e, smi=smi: e.tensor_tensor(out=smi[:, 3, :], in0=smi[:, 2, :], in1=smi[:, 2, :], op=ALU.mult), [smi.res], [smi.res])
                dve(lambda e, smi=smi: e.scalar_tensor_tensor(out=smi[:, 4, :], in0=smi[:, 1, :], scalar=1.0 / 256, in1=smi[:, 3, :],
                                                              op0=ALU.mult, op1=ALU.subtract), [smi.res], [smi.res])
                act(smi[:, 5, :], smi[:, 4, :], AF.Sqrt, [smi.res, cst.res], [smi.res], bias=cst[:, 0:1], scale=1.0)
                dve(lambda e, smi=smi: e.reciprocal(out=smi[:, 6, :], in_=smi[:, 5, :]), [smi.res], [smi.res])
                for hd in range(4):
                    dve(lambda e, hd=hd, smi=smi, hmi=hmi: e.tensor_scalar(
                        out=hmi[:, hd * 256:(hd + 1) * 256], in0=hmi[:, hd * 256:(hd + 1) * 256],
                        scalar1=smi[:, 2, hd:hd + 1], scalar2=smi[:, 6, hd:hd + 1], op0=ALU.subtract, op1=ALU.mult),
                        [hmi.res, smi.res], [hmi.res])
                ml = mls[j % 2]
                dve(lambda e, ml=ml, hmi=hmi: e.tensor_tensor(out=ml[:], in0=hmi[:], in1=gz[:], op=ALU.mult),
                    [hmi.res, gz.res], [ml.res])
                pt = psTb[j % 2]
                for c in range(8):
                    p.add("pe", lambda e, pt=pt, ml=ml, c=c: e.transpose(pt[:, c * 128:(c + 1) * 128], ml[:, c * 128:(c + 1) * 128], ident[:]),
                          reads=[ml.res, ident.res], writes=[pt.res])
                act(mx[:, :, tk], pt[:, :].rearrange("p (c t) -> p c t", c=8), AF.Copy, [pt.res], [mx.res])
            dma("sp", mixT_d[b, 8:16, :, tb * 512:(tb + 1) * 512].rearrange("c p t -> p c t"), mx[:],
                reads=[mx.res], writes=[DR("mixT", b, 8 + tb)])

    def phase_E(L, b):
        ar = Arena()
        wo = ar.get("wo", [128, 16, D], BF16)
        mxl = [ar.get("mxl%d" % i, [128, 16, 512], BF16) for i in range(2)]
        xr = [ar.get("xr%d" % i, [128, 4, D], F32) for i in range(2)]
        xo = [ar.get("xo%d" % i, [128, 4, D], F32) for i in range(2)]
        gf = ar.get("gf", [128, D], F32)
        junk = ar.get("junkE", [128, D], F32)
        stt = ar.get("sttE", [128, 3, 32], F32)
        ps = psum_views()
        last = (L == DEPTH - 1)
        src = x_d if L == 0 else x1_d
        dma("pool", wo[:], wout_d[L].rearrange("(k p) c -> p k c", p=128), writes=[wo.res])
        if last:
            dma("sp", gf[:], fing_d.partition_broadcast(128), writes=[gf.res])

        def load(tb):
            i = tb % 2
            cs = slice(tb * 512, (tb + 1) * 512)
            dma("sp", mxl[i][:], mixT_d[b, :, :, cs].rearrange("c p t -> p c t"),
                reads=[DR("mixT", b, k_) for k_ in range(16)], writes=[mxl[i].res])
            dma("sp", xr[i][:], src[b, cs, :].rearrange("(n p) c -> p n c", p=128),
                reads=[DR("x1", b, tb)] if L > 0 else [], writes=[xr[i].res])

        load(0)
        k_ = 0
        for tb in range(8):
            i = tb % 2
            if tb + 1 < 8:
                load(tb + 1)
            for n in range(4):
                for half in range(2):
                    bk = ps[k_ % 4]
                    k_ += 1
                    for kc in range(16):
                        mm(bk[:, :], mxl[i][:, kc, n * 128:(n + 1) * 128], wo[:, kc, half * 512:(half + 1) * 512],
                           kc == 0, kc == 15, [mxl[i].res, wo.res], [bk.res])
                    dve(lambda e, i=i, n=n, half=half, bk=bk: e.tensor_tensor(
                        out=xo[i][:, n, half * 512:(half + 1) * 512], in0=bk[:, :],
                        in1=xr[i][:, n, half * 512:(half + 1) * 512], op=ALU.add), [bk.res, xr[i].res], [xo[i].res])
                if last:
                    ti = tb * 4 + n
                    act(junk[:], xo[i][:, n, :], AF.Square, [xo[i].res], [junk.res, stt.res], accum=stt[:, 0, ti:ti + 1])
                    act(stt[:, 1, ti:ti + 1], stt[:, 0, ti:ti + 1], AF.Sqrt, [stt.res, cst.res], [stt.res],
                        bias=cst[:, 0:1], scale=1.0 / D)
                    dve(lambda e, ti=ti: e.reciprocal(out=stt[:, 2, ti:ti + 1], in_=stt[:, 1, ti:ti + 1]), [stt.res], [stt.res])
                    dve(lambda e, i=i, n=n, ti=ti: e.scalar_tensor_tensor(
                        out=xo[i][:, n, :], in0=xo[i][:, n, :], scalar=stt[:, 2, ti:ti + 1], in1=gf[:],
                        op0=ALU.mult, op1=ALU.mult), [xo[i].res, stt.res, gf.res], [xo[i].res])
            dst = y_d if last else x1_d
            dma("sp", dst[b, tb * 512:(tb + 1) * 512, :].rearrange("(n p) c -> p n c", p=128), xo[i][:],
                reads=[xo[i].res], writes=[] if last else [DR("x1", b, tb)])

    fns = {"A": phase_A, "B": phase_B, "C": phase_C, "D": phase_D, "E": phase_E}
    for L in layers:
        for b in seqs:
            for ph in phases:
                fns[ph](L, b)
                p.barrier()
    p.emit()
    return nc


def host_consts():
    ident = np.eye(128, dtype=np.float32).astype(ml_dtypes.bfloat16)
    tri = np.triu(np.ones((128, 128), np.float32))
    slopes = np.exp2(-8.0 * np.arange(1, 9, dtype=np.float32) / 8.0)
    i = np.arange(128)[:, None]
    j = np.arange(128)[None, :]
    am = np.zeros((128, 48, 128), np.float32)
    for h in range(8):
        for di, d_ in enumerate(DILS):
            cur = np.where(j >= i, -slopes[h] * d_ * (j - i), NEG)
            prev = np.where(j <= i, -slopes[h] * d_ * (128 + j - i), NEG)
            am[:, (h * 3 + di) * 2, :] = cur
            am[:, (h * 3 + di) * 2 + 1, :] = prev
    return ident, tri, am.astype(ml_dtypes.bfloat16)


def host_layout(inputs):
    f = lambda a: np.ascontiguousarray(np.asarray(a, dtype=np.float32))
    conv_w = f(inputs["conv_w"])
    cdiag = np.zeros((DEPTH, 128, 8, 4, 128), np.float32)
    pidx = np.arange(128)
    for L in range(DEPTH):
        for c in range(8):
            for w in range(4):
                cdiag[L, pidx, c, w, pidx] = conv_w[L, w, c * 128 + pidx]
    cb = np.ascontiguousarray(f(inputs["conv_b"]).reshape(DEPTH, 8, 128).transpose(0, 2, 1))

    def bd(w):
        w = f(w)
        out = np.zeros((DEPTH, 128, 8, 128), np.float32)
        for L in range(DEPTH):
            for c in range(8):
                for n in range(32):
                    out[L, n * 4:(n + 1) * 4, c, n * 4:(n + 1) * 4] = w[L, c * 32 + n]
        return out

    ident, tri, am = host_consts()
    common = {
        "norm_g": f(inputs["norm_g"]), "w_in": f(inputs["w_in"]), "cdiag": cdiag, "cb": cb,
        "wq_bd": bd(inputs["w_qm"]), "wk_bd": bd(inputs["w_km"]), "wv_bd": bd(inputs["w_vm"]),
        "w_if": np.ascontiguousarray(np.pad(f(inputs["w_if"]).reshape(DEPTH, 24, 128, 8).transpose(0, 2, 1, 3), ((0, 0), (0, 0), (0, 0), (0, 24)))), "b_i": np.ascontiguousarray(np.broadcast_to(f(inputs["b_i"])[:, None, :], (DEPTH, 128, 4))), "b_f": np.ascontiguousarray(np.broadcast_to(f(inputs["b_f"])[:, None, :], (DEPTH, 128, 4))),
        "hn_g": f(inputs["hn_g"]), "w_out": f(inputs["w_out"]), "final_g": f(inputs["final_g"]),
        "ident": ident, "tri": tri, "amask": am,
    }
    return common


def kernel(**inputs):
    x = np.ascontiguousarray(np.asarray(inputs["x"], dtype=np.float32))
    common = host_layout(inputs)
    nc = build()
    in_maps = []
    for c in range(8):
        m = dict(common)
        m["x"] = x[c * NSEQ:(c + 1) * NSEQ]
        in_maps.append(m)
    res = run_bass_kernel_spmd(nc, in_maps, core_ids=list(range(8)))
    return np.concatenate([r["y"] for r in res.results], axis=0).astype(np.float32)
```

```python
import math
import os
import numpy as np
import ml_dtypes
import concourse.bass as bass
import concourse.mybir as mybir
from concourse.bass_utils import run_bass_kernel_spmd

F32 = mybir.dt.float32
BF16 = mybir.dt.bfloat16
AF = mybir.ActivationFunctionType
ALU = mybir.AluOpType
AX = mybir.AxisListType

ENGS = ["pe", "act", "dve", "pool", "sp"]
S = 4096
D = 1024
DIN = 7168
NSEQ = 2
DEPTH = 2
EPS = 1e-6
NEG = -30000.0
DILS = (1, 4, 16)


class Res:
    __slots__ = ("name", "writers", "readers", "excl")

    def __init__(self, name="", excl=False):
        self.name = name
        self.writers = {}
        self.readers = {}
        self.excl = excl


class Op:
    __slots__ = ("eng", "fn", "deps", "dma", "pos", "qidx", "needs_inc", "sig", "waits")


class Prog:
    def __init__(self, nc, K=8):
        self.nc = nc
        self.K = K
        self.ops = {e: [] for e in ENGS}
        self.ndma = {e: 0 for e in ENGS}
        self.pending = {e: [] for e in ENGS}

    def add(self, eng, fn, reads=(), writes=(), dma=False):
        o = Op()
        o.eng, o.fn, o.dma = eng, fn, dma
        o.pos = len(self.ops[eng])
        o.needs_inc = False
        o.sig = None
        o.waits = []
        o.qidx = -1
        if dma:
            o.qidx = self.ndma[eng]
            self.ndma[eng] += 1
        deps = []
        for r in reads:
            deps.extend(r.writers.values())
            if r.excl:
                deps.extend(o2 for o2 in r.readers.values() if o2.eng != eng)
        for w in writes:
            deps.extend(w.readers.values())
            deps.extend(w.writers.values())
        if self.pending[eng]:
            deps.extend(self.pending[eng])
            self.pending[eng] = []
        o.deps = deps
        k = (eng, o.qidx % self.K) if dma else (eng,)
        for r in reads:
            r.readers[k] = o
        for w in writes:
            w.writers[k] = o
        self.ops[eng].append(o)
        return o

    def barrier(self):
        last = []
        for e in ENGS:
            ops = self.ops[e]
            if not ops:
                continue
            last.append(ops[-1])
            seen = set()
            for o in reversed(ops):
                if o.dma:
                    s = o.qidx % self.K
                    if s not in seen:
                        seen.add(s)
                        last.append(o)
                    if len(seen) == self.K:
                        break
        for e in ENGS:
            self.pending[e] = list(last)

    def emit(self):
        nc = self.nc
        K = self.K
        from contextlib import ExitStack
        with ExitStack() as st:
            esem = {e: st.enter_context(nc.semaphore("s_" + e)) for e in ENGS}
            rsem = {e: [st.enter_context(nc.semaphore("r_%s%d" % (e, i))) for i in range(K)]
                    for e in ENGS if self.ndma[e] > 0}
            for e in ENGS:
                known = {}
                for o in self.ops[e]:
                    need = {}
                    for d in o.deps:
                        if d is o:
                            continue
                        if d.dma:
                            key = ("d", d.eng, d.qidx % K)
                            val = d.qidx // K + 1
                        else:
                            if d.eng == e and e == "pe":
                                continue
                            key = ("c", d.eng)
                            val = d.pos
                        if key not in need or need[key][0] < val:
                            need[key] = (val, d)
                    if o.dma and o.qidx >= K:
                        key = ("d", e, o.qidx % K)
                        val = o.qidx // K
                        if key not in need or need[key][0] < val:
                            need[key] = (val, None)
                    for key, (val, d) in need.items():
                        if known.get(key, -1) >= val:
                            continue
                        known[key] = val
                        if key[0] == "c":
                            d.needs_inc = True
                            o.waits.append(("c", d))
                        else:
                            o.waits.append(("d", key[1], key[2], val))
            for e in ENGS:
                cnt = 0
                for o in self.ops[e]:
                    if (not o.dma) and o.needs_inc:
                        cnt += 1
                        o.sig = cnt
            final = []
            for e in rsem:
                n = self.ndma[e]
                for s in range(K):
                    c = (n - s + K - 1) // K if n > s else 0
                    if c > 0:
                        final.append((rsem[e][s], 16 * c))

            def run(e, eng):
                for o in self.ops[e]:
                    for w in o.waits:
                        if w[0] == "c":
                            eng.wait_ge(esem[w[1].eng], w[1].sig)
                        else:
                            eng.wait_ge(rsem[w[1]][w[2]], 16 * w[3])
                    ins = o.fn(eng)
                    if o.dma:
                        ins.then_inc(rsem[e][o.qidx % K], 16)
                    elif o.needs_inc:
                        ins.then_inc(esem[e], 1)
                if e == "sp":
                    for sem, v in final:
                        eng.wait_ge(sem, v)

            with nc.Block() as block:
                @block.tensor
                def _(pe):
                    run("pe", pe)

                @block.scalar
                def _(act):
                    run("act", act)

                @block.vector
                def _(dve):
                    run("dve", dve)

                @block.gpsimd
                def _(pool):
                    run("pool", pool)

                @block.sync
                def _(sp):
                    run("sp", sp)


class T:
    __slots__ = ("ap", "res")

    def __init__(self, ap, name="", excl=False):
        self.ap = ap
        self.res = Res(name, excl)

    def __getitem__(self, k):
        return self.ap[k]


def build(layers=(0, 1), seqs=(0, 1), phases="ABCDE", dbg=False):
    nc = bass.Bass("TRN2", target_bir_lowering=False)
    p = Prog(nc)
    kind_dbg = "ExternalOutput" if dbg else "Internal"

    def din(name, shape, dt=F32):
        return nc.dram_tensor(name, list(shape), dt, kind="ExternalInput").ap()

    x_d = din("x", [NSEQ, S, D])
    normg_d = din("norm_g", [DEPTH, D])
    win_d = din("w_in", [DEPTH, D, DIN])
    cdiag_d = din("cdiag", [DEPTH, 128, 8, 4, 128])
    cb_d = din("cb", [DEPTH, 128, 8])
    wq_d = din("wq_bd", [DEPTH, 128, 8, 128])
    wk_d = din("wk_bd", [DEPTH, 128, 8, 128])
    wv_d = din("wv_bd", [DEPTH, 128, 8, 128])
    wif_d = din("w_if", [DEPTH, 128, 24, 32])
    bi_d = din("b_i", [DEPTH, 128, 4])
    bf_d = din("b_f", [DEPTH, 128, 4])
    hng_d = din("hn_g", [DEPTH, D])
    wout_d = din("w_out", [DEPTH, 2 * D, D])
    fing_d = din("final_g", [D])
    ident_d = din("ident", [128, 128], BF16)
    tri_d = din("tri", [128, 128])
    amask_d = din("amask", [128, 48, 128], BF16)
    y_d = nc.dram_tensor("y", [NSEQ, S, D], F32, kind="ExternalOutput").ap()

    def dscr(name, shape, dt=BF16):
        return nc.dram_tensor(name, list(shape), dt, kind=kind_dbg).ap()

    qT_d = dscr("qT_s", [NSEQ, 8, 128, S])
    kT_d = dscr("kT_s", [NSEQ, 8, 128, S])
    szT_d = dscr("szT_s", [NSEQ, 8, 128, S])
    v3_d = dscr("v3_s", [NSEQ, 3, 8, 128, 32, 128])
    xmT_d = dscr("xmT_s", [NSEQ, 8, 128, 8 + S])
    som_d = dscr("som_s", [NSEQ, S, D])
    szm_d = dscr("szm_s", [NSEQ, S, D])
    mixT_d = dscr("mixT_s", [NSEQ, 16, 128, S])
    qmT_d = dscr("qmT_s", [NSEQ, 8, 128, S])
    kmT_d = dscr("kmT_s", [NSEQ, 8, 128, S])
    km_d = dscr("km_s", [NSEQ, S, D])
    vm_d = dscr("vm_s", [NSEQ, S, D])
    x1_d = dscr("x1_s", [NSEQ, S, D], F32)
    gp_dbg = dscr("gp_s", [NSEQ, 128, 4, 128], F32) if dbg else None

    dres = {}

    def DR(*key):
        if key not in dres:
            dres[key] = Res(str(key))
        return dres[key]

    def persist(name, shape, dt):
        return T(nc.alloc_sbuf_tensor(name, list(shape), dt), name)

    ident = persist("ident_sb", [128, 128], BF16)
    tri = persist("tri_sb", [128, 128], F32)
    ones_f = persist("ones_f", [128, 128], F32)
    ones_b = persist("ones_b", [128, 128], BF16)
    tri_b = persist("tri_b", [128, 128], BF16)
    amask = persist("amask_sb", [128, 48, 128], BF16)
    cst = persist("cst", [128, 8], F32)
    gp = persist("gp", [128, 4, 128], F32)
    ARENA_W = 47 * 1024
    arena = nc.alloc_sbuf_tensor("arena", [128, ARENA_W], F32)
    banks = [nc.alloc_psum_tensor("bank%d" % i, [128, 512], F32) for i in range(8)]

    class Arena:
        def __init__(self):
            self.off = 0

        def get(self, name, shape, dt):
            n = 1
            for s_ in shape[1:]:
                n *= s_
            nbytes = n * (4 if dt == F32 else 2)
            nbytes = (nbytes + 63) // 64 * 64
            v = arena[:, self.off // 4:(self.off + nbytes) // 4]
            if dt != F32:
                v = v.bitcast(dt)
            v = v[:, 0:n]
            if len(shape) == 3:
                v = v.rearrange("p (a b) -> p a b", a=shape[1])
            elif len(shape) == 4:
                v = v.rearrange("p (a b c) -> p a b c", a=shape[1], b=shape[2])
            self.off += nbytes
            assert self.off <= ARENA_W * 4, (name, self.off)
            return T(v, name)

    def psum_views(dt=F32):
        out = []
        for b_ in banks:
            v = b_[:, :]
            if dt != F32:
                v = v.bitcast(dt)
            out.append(T(v, "bank", True))
        return out

    def dma(q, out, in_, reads=(), writes=()):
        p.add(q, lambda e: e.dma_start(out=out, in_=in_), reads=reads, writes=writes, dma=True)

    def mm(out, lhsT, rhs, start, stop, reads, writes, skip=False):
        if skip:
            p.add("pe", lambda e: e.matmul(out, lhsT=lhsT, rhs=rhs, start=start, stop=stop,
                                           skip_group_check=True), reads=reads, writes=writes)
        else:
            p.add("pe", lambda e: e.matmul(out, lhsT=lhsT, rhs=rhs, start=start, stop=stop),
                  reads=reads, writes=writes)

    def act(out, in_, func, reads, writes, bias=None, scale=1.0, accum=None):
        kw = {}
        if bias is not None:
            kw["bias"] = bias
        if accum is not None:
            kw["accum_out"] = accum
        p.add("act", lambda e: e.activation(out=out, in_=in_, func=func, scale=scale, **kw),
              reads=reads, writes=writes)

    def dve(fn, reads, writes):
        p.add("dve", fn, reads=reads, writes=writes)

    dma("sp", ident[:], ident_d, writes=[ident.res])
    dma("sp", tri[:], tri_d, writes=[tri.res])
    dma("sp", amask[:], amask_d, writes=[amask.res])
    dve(lambda e: e.memset(ones_f[:], 1.0), [], [ones_f.res])
    dve(lambda e: e.memset(ones_b[:], 1.0), [], [ones_b.res])
    dve(lambda e: e.tensor_copy(out=tri_b[:], in_=tri[:]), [tri.res], [tri_b.res])
    dve(lambda e: e.memset(cst[:, 0:1], EPS), [], [cst.res])
    dve(lambda e: e.memset(cst[:, 1:2], -math.log(16.0)), [], [cst.res])
    dve(lambda e: e.memset(cst[:, 2:3], 1.0), [], [cst.res])

    zpad = persist("zpad", [128, 8, 8], BF16)
    dve(lambda e: e.memset(zpad[:], 0.0), [], [zpad.res])
    for b_ in range(NSEQ):
        dma("sp", xmT_d[b_, :, :, 0:8].rearrange("c p t -> p c t"), zpad[:], reads=[zpad.res], writes=[DR("xmTpad", b_)])

    evac_rr = [0]

    def copy_evac(out, in_, reads, writes, scale=None):
        evac_rr[0] ^= 1
        if evac_rr[0]:
            act(out, in_, AF.Copy, reads, writes, scale=(1.0 if scale is None else scale))
        else:
            if scale is None:
                dve(lambda e: e.tensor_copy(out=out, in_=in_), reads, writes)
            else:
                dve(lambda e: e.tensor_scalar_mul(out=out, in0=in_, scalar1=scale), reads, writes)

    def phase_A(L, b):
        ar = Arena()
        hT = ar.get("hT", [128, 8, S], BF16)
        hres = [Res("hT%d" % i) for i in range(8)]
        xin = [ar.get("xin%d" % i, [128, 4, D], F32) for i in range(2)]
        xn = [ar.get("xn%d" % i, [128, D], BF16) for i in range(2)]
        junk = ar.get("junk", [128, D], BF16)
        W = [ar.get("W%d" % i, [128, 8, 512], BF16) for i in range(2)]
        ofm = [ar.get("ofm%d" % i, [128, S], BF16) for i in range(2)]
        otm = [ar.get("otm%d" % i, [128, 4, 512], BF16) for i in range(2)]
        vst = [ar.get("vst%d" % i, [128, 4, 32, 128], BF16) for i in range(1)]
        gn = ar.get("gn", [128, D], F32)
        stt = ar.get("stt", [128, 3, 32], F32)
        ps = psum_views()
        psT = [T(banks[i][:, :].bitcast(BF16), "psT", True) for i in range(2)]

        src = x_d if L == 0 else x1_d
        dma("sp", gn[:], normg_d[L].partition_broadcast(128), writes=[gn.res])

        def load_x(tq):
            dma("sp", xin[tq % 2][:], src[b, tq * 512:(tq + 1) * 512, :].rearrange("(n p) c -> p n c", p=128),
                reads=[DR("x1", b, tq)] if L > 0 else [], writes=[xin[tq % 2].res])

        jobs = []
        for wb in (0, 1):
            jobs.append(("fm", wb, AF.Copy, None))
        for wb in (2, 3):
            jobs.append(("fm", wb, AF.Copy, None))
        for wb in (8, 9):
            jobs.append(("fm", wb, AF.Copy, None))
        for di in range(3):
            for wb in (4, 5):
                jobs.append(("v", wb, AF.Copy, di))
        for wb in (6, 7):
            jobs.append(("fm", wb, AF.Silu, None))
        for wb in (12, 13):
            jobs.append(("tm", wb, AF.Silu, None))
        for wb in (10, 11):
            jobs.append(("tm", wb, AF.Sigmoid, None))
        wl = []
        for jb in jobs:
            if not wl or wl[-1] != jb[1]:
                wl.append(jb[1])
        wl_state = {"next": 0}

        def load_w():
            i = wl_state["next"]
            if i >= len(wl):
                return
            wb = wl[i]
            dma("pool", W[i % 2][:], win_d[L, :, wb * 512:(wb + 1) * 512].rearrange("(kc p) j -> p kc j", p=128),
                writes=[W[i % 2].res])
            wl_state["next"] = i + 1

        load_x(0)
        load_w()
        for tq in range(8):
            if tq + 1 < 8:
                load_x(tq + 1)
            xi = xin[tq % 2]
            for n in range(4):
                ti = tq * 4 + n
                xb = xn[ti % 2]
                act(junk[:], xi[:, n, :], AF.Square, [xi.res], [junk.res, stt.res], accum=stt[:, 0, ti:ti + 1])
                act(stt[:, 1, ti:ti + 1], stt[:, 0, ti:ti + 1], AF.Sqrt, [stt.res, cst.res], [stt.res],
                    bias=cst[:, 0:1], scale=1.0 / D)
                dve(lambda e, ti=ti: e.reciprocal(out=stt[:, 2, ti:ti + 1], in_=stt[:, 1, ti:ti + 1]),
                    [stt.res], [stt.res])
                dve(lambda e, xi=xi, n=n, ti=ti, xb=xb: e.scalar_tensor_tensor(
                    out=xb[:], in0=xi[:, n, :], scalar=stt[:, 2, ti:ti + 1], in1=gn[:],
                    op0=ALU.mult, op1=ALU.mult), [xi.res, stt.res, gn.res], [xb.res])
                pt = psT[ti % 2]
                for kc in range(8):
                    p.add("pe", lambda e, pt=pt, xb=xb, kc=kc: e.transpose(
                        pt[:, kc * 128:(kc + 1) * 128], xb[:, kc * 128:(kc + 1) * 128], ident[:]),
                        reads=[xb.res, ident.res], writes=[pt.res])
                copy_evac(hT[:, :, ti * 128:(ti + 1) * 128],
                          pt[:, :].rearrange("p (k t) -> p k t", k=8), [pt.res], [hres[tq]])
        pb = [2]

        def next_bank():
            r = ps[pb[0]]
            pb[0] = pb[0] + 1 if pb[0] < 7 else 2
            return r

        fm_i = [0]
        tm_i = [0]
        wcur = -1
        wprev = None
        for (kind, wb, func, di) in jobs:
            if wprev != wb:
                wcur += 1
                wprev = wb
                load_w()
            Wt = W[wcur % 2]
            if kind == "fm":
                for g in range(4):
                    col = wb * 512 + g * 128
                    if col < 1024:
                        dst, scale = qT_d[b, col // 128], 128 ** -0.5
                        dr = DR("qT", b, col // 128)
                    elif col < 2048:
                        dst, scale = kT_d[b, (col - 1024) // 128], None
                        dr = DR("kT", b, (col - 1024) // 128)
                    elif col < 4096:
                        dst, scale = szT_d[b, (col - 3072) // 128], None
                        dr = DR("szT", b, (col - 3072) // 128)
                    else:
                        dst, scale = xmT_d[b, (col - 4096) // 128][:, 8:8 + S], None
                        dr = DR("xmT", b, (col - 4096) // 128)
                    ob = ofm[fm_i[0] % 2]
                    fm_i[0] += 1
                    for tb in range(8):
                        bk = next_bank()
                        for kc in range(8):
                            mm(bk[:, :], Wt[:, kc, g * 128:(g + 1) * 128], hT[:, kc, tb * 512:(tb + 1) * 512],
                               kc == 0, kc == 7, [Wt.res, hres[tb]], [bk.res])
                        if func == AF.Copy:
                            copy_evac(ob[:, tb * 512:(tb + 1) * 512], bk[:, :], [bk.res], [ob.res], scale=scale)
                        else:
                            act(ob[:, tb * 512:(tb + 1) * 512], bk[:, :], func, [bk.res], [ob.res])
                    dma("sp", dst, ob[:], reads=[ob.res], writes=[dr])
            elif kind == "tm":
                dst_t, nm = (szm_d, "szm") if wb >= 12 else (som_d, "som")
                c0 = (wb % 2) * 512
                for ti in range(32):
                    ob = otm[tm_i[0] % 2]
                    bk = next_bank()
                    for kc in range(8):
                        mm(bk[:, :], hT[:, kc, ti * 128:(ti + 1) * 128], Wt[:, kc, :], kc == 0, kc == 7,
                           [Wt.res, hres[ti // 4]], [bk.res])
                    act(ob[:, ti % 4, :], bk[:, :], func, [bk.res], [ob.res])
                    if ti % 4 == 3:
                        tq = ti // 4
                        dma("sp", dst_t[b, tq * 512:(tq + 1) * 512, c0:c0 + 512].rearrange("(n p) c -> p n c", p=128),
                            ob[:], reads=[ob.res], writes=[DR(nm, b, tq)])
                        tm_i[0] += 1
            else:
                d_ = DILS[di]
                hh0 = (wb % 2) * 4
                vs = vst[0]
                for blk in range(32):
                    nblk = 32 // d_
                    r, n = blk // nblk, blk % nblk
                    base = r + d_ * 128 * n
                    bk = next_bank()
                    for kc in range(8):
                        mm(bk[:, :], hT[:, kc, base:base + d_ * 127 + 1:d_], Wt[:, kc, :], kc == 0, kc == 7,
                           [Wt.res] + [hres[i] for i in range(base // 512, min(8, (base + d_ * 128 - 1) // 512 + 1))],
                           [bk.res])
                    copy_evac(vs[:, :, blk, :], bk[:, :].rearrange("p (h c) -> p h c", h=4), [bk.res], [vs.res])
                for hh in range(4):
                    dma("sp", v3_d[b, di, hh0 + hh], vs[:, hh, :, :], reads=[vs.res], writes=[DR("v3", b, hh0 + hh)])

    def phase_B(L, b):
        ar = Arena()
        qT = [ar.get("qT%d" % i, [128, S], BF16) for i in range(2)]
        kT = [ar.get("kT%d" % i, [128, S], BF16) for i in range(2)]
        szT = [ar.get("szT%d" % i, [128, S], BF16) for i in range(2)]
        v3 = [[ar.get("v%d_%d" % (di, i), [128, 32, 128], BF16) for di in range(3)] for i in range(2)]
        accn = ar.get("accn", [128, S], F32)
        accd = ar.get("accd", [128, S], F32)
        att = [ar.get("att%d" % i, [128, S], BF16) for i in range(2)]
        PT = [ar.get("PT%d" % i, [128, 256], BF16) for i in range(4)]
        t1 = [ar.get("t1_%d" % i, [128, 512], F32) for i in range(2)]
        t2 = [ar.get("t2_%d" % i, [128, 512], F32) for i in range(2)]
        ps = psum_views()
        sc_i = [0]
        acc_i = [0]

        def load_head(h):
            i = h % 2
            dma("sp", qT[i][:], qT_d[b, h], reads=[DR("qT", b, h)], writes=[qT[i].res])
            dma("sp", kT[i][:], kT_d[b, h], reads=[DR("kT", b, h)], writes=[kT[i].res])
            dma("sp", szT[i][:], szT_d[b, h], reads=[DR("szT", b, h)], writes=[szT[i].res])
            for di in range(3):
                dma("sp", v3[i][di][:], v3_d[b, di, h], reads=[DR("v3", b, h)], writes=[v3[i][di].res])

        pend = []
        LAG = 2

        def drain(keep):
            while sum(1 for k_, _f in pend if k_ == "pv") > keep:
                pend.pop(0)[1]()
            while pend and pend[0][0] == "ev":
                pend.pop(0)[1]()

        def qblock(h, di, qsl, ksl_prev, ksl_cur, blk_prev, blk_cur, outn, outd, first, last):
            i = h % 2
            sb = ps[sc_i[0] % 4]
            pt = PT[sc_i[0] % 4]
            sc_i[0] += 1
            mi = (h * 3 + di) * 2
            c0 = 0 if ksl_prev is not None else 128
            if ksl_prev is not None:
                mm(sb[:, 0:128], kT[i][:, ksl_prev], qT[i][:, qsl], True, False, [kT[i].res, qT[i].res], [sb.res])
                mm(sb[:, 0:128], ident[:], amask[:, mi + 1, :], False, True, [ident.res, amask.res], [sb.res])
            mm(sb[:, 128:256], kT[i][:, ksl_cur], qT[i][:, qsl], True, False, [kT[i].res, qT[i].res], [sb.res])
            mm(sb[:, 128:256], ident[:], amask[:, mi, :], False, True, [ident.res, amask.res], [sb.res])
            act(pt[:, c0:256], sb[:, c0:256], AF.Exp, [sb.res], [pt.res])
            vv = v3[i][di]

            def part2():
                for (o_, lw) in ((outn, None), (outd, ones_b)):
                    seq = []
                    if ksl_prev is not None:
                        seq.append((blk_prev, pt[:, 0:128]))
                    seq.append((blk_cur, pt[:, 128:256]))
                    for si, (bk_, rhs) in enumerate(seq):
                        lhs = vv[:, bk_, :] if lw is None else lw[:]
                        st_ = first and si == 0
                        sp_ = last and si == len(seq) - 1
                        mm(o_[0], lhs, rhs, st_, sp_, [vv.res if lw is None else lw.res, pt.res], [o_[1]], skip=True)

            pend.append(("pv", part2))
            drain(LAG)

        def evac16(bn, bd, n, rq):
            def f():
                for (bk_, acc, eng) in ((bn, accn, "act"), (bd, accd, "dve")):
                    dst = acc[:, n * 2048:(n + 1) * 2048].rearrange("p (j r) -> p j r", r=16)[:, :, rq * 4:rq * 4 + 4]
                    dst = dst.rearrange("p j r -> p r j")
                    srcv = bk_[:, :].rearrange("p (r j) -> p r j", r=4)
                    if eng == "act":
                        act(dst, srcv, AF.Copy, [bk_.res], [acc.res])
                    else:
                        dve(lambda e, dst=dst, srcv=srcv: e.tensor_copy(out=dst, in_=srcv), [bk_.res], [acc.res])
            return f

        def combine(bn, bd, tb, i):
            def f():
                a1, a2 = t1[tb % 2], t2[tb % 2]
                cs = slice(tb * 512, (tb + 1) * 512)
                dve(lambda e: e.tensor_tensor(out=a1[:], in0=bn[:, :], in1=accn[:, cs], op=ALU.add),
                    [bn.res, accn.res], [a1.res])
                dve(lambda e: e.tensor_tensor(out=a2[:], in0=bd[:, :], in1=accd[:, cs], op=ALU.add),
                    [bd.res, accd.res], [a2.res])
                dve(lambda e: e.reciprocal(out=a2[:], in_=a2[:]), [a2.res], [a2.res])
                dve(lambda e: e.tensor_tensor(out=a1[:], in0=a1[:], in1=a2[:], op=ALU.mult),
                    [a1.res, a2.res], [a1.res])
                dve(lambda e: e.tensor_tensor(out=att[i][:, cs], in0=a1[:], in1=szT[i][:, cs], op=ALU.mult),
                    [a1.res, szT[i].res], [att[i].res])
            return f

        load_head(0)
        for h in range(8):
            if h + 1 < 8:
                load_head(h + 1)
            i = h % 2
            for n in range(2):
                for rq in range(4):
                    bn = ps[4 + acc_i[0] % 2]
                    bd = ps[6 + acc_i[0] % 2]
                    acc_i[0] += 1
                    for rr in range(4):
                        r = rq * 4 + rr
                        qsl = slice(n * 2048 + r, n * 2048 + r + 16 * 127 + 1, 16)
                        kprev = slice((n - 1) * 2048 + r, (n - 1) * 2048 + r + 16 * 127 + 1, 16) if n >= 1 else None
                        qblock(h, 2, qsl, kprev, qsl, r * 2 + n - 1, r * 2 + n,
                               (bn[:, rr * 128:(rr + 1) * 128], bn.res), (bd[:, rr * 128:(rr + 1) * 128], bd.res),
                               True, True)
                    pend.append(("ev", evac16(bn, bd, n, rq)))
            for tb in range(8):
                bn = ps[4 + acc_i[0] % 2]
                bd = ps[6 + acc_i[0] % 2]
                acc_i[0] += 1
                for qb in range(4):
                    n = tb * 4 + qb
                    qsl = slice(n * 128, (n + 1) * 128)
                    kprev = slice((n - 1) * 128, n * 128) if n >= 1 else None
                    qblock(h, 0, qsl, kprev, qsl, n - 1, n,
                           (bn[:, qb * 128:(qb + 1) * 128], bn.res), (bd[:, qb * 128:(qb + 1) * 128], bd.res),
                           qb == 0, False)
                for r in range(4):
                    qsl = slice(tb * 512 + r, tb * 512 + r + 4 * 127 + 1, 4)
                    kprev = slice((tb - 1) * 512 + r, (tb - 1) * 512 + r + 4 * 127 + 1, 4) if tb >= 1 else None
                    qblock(h, 1, qsl, kprev, qsl, r * 8 + tb - 1, r * 8 + tb,
                           (bn[:, r:r + 4 * 127 + 1:4], bn.res), (bd[:, r:r + 4 * 127 + 1:4], bd.res), False, r == 3)
                pend.append(("ev", combine(bn, bd, tb, i)))
            drain(0)
            dma("sp", mixT_d[b, h], att[i][:], reads=[att[i].res], writes=[DR("mixT", b, h)])

    def phase_C(L, b):
        ar = Arena()
        xf = [ar.get("xf%d" % i, [128, 8, 520], BF16) for i in range(2)]
        xc = [ar.get("xc%d" % i, [128, 8, 512], BF16) for i in range(2)]
        qs = [ar.get("qs%d" % i, [128, 8, 512], BF16) for i in range(2)]
        ks = [ar.get("ks%d" % i, [128, 8, 512], BF16) for i in range(2)]
        vs = [ar.get("vs%d" % i, [128, 8, 512], BF16) for i in range(2)]
        ktm = [ar.get("ktm%d" % i, [128, 4, D], BF16) for i in range(2)]
        vtm = [ar.get("vtm%d" % i, [128, 4, D], BF16) for i in range(2)]
        cdg = ar.get("cdg", [128, 8, 4, 128], BF16)
        wq = ar.get("wq", [128, 8, 128], BF16)
        wk = ar.get("wk", [128, 8, 128], BF16)
        wv = ar.get("wv", [128, 8, 128], BF16)
        wif = ar.get("wif", [128, 24, 32], BF16)
        cb = ar.get("cb", [128, 8], F32)
        bib = ar.get("bib", [128, 4], F32)
        bfb = ar.get("bfb", [128, 4], F32)
        graw = ar.get("graw", [128, 32, 8], F32)
        gt = ar.get("gt", [128, 4, 128], F32)
        xh = ar.get("xh", [128, 3, 128], BF16)
        xr_ = ar.get("xr_", [128, 2, 128], F32)
        ps = psum_views()

        for c_ in range(8):
            dma("pool", cdg[:, c_, :, :], cdiag_d[L, :, c_, :, :], writes=[cdg.res])
        dma("pool", wq[:], wq_d[L], writes=[wq.res])
        dma("pool", wk[:], wk_d[L], writes=[wk.res])
        dma("pool", wv[:], wv_d[L], writes=[wv.res])
        dma("pool", wif[:], wif_d[L], writes=[wif.res])
        dma("sp", cb[:], cb_d[L], writes=[cb.res])
        dma("sp", bib[:], bi_d[L], writes=[bib.res])
        dma("sp", bfb[:], bf_d[L], writes=[bfb.res])
        dve(lambda e: e.tensor_scalar_mul(out=bfb[:], in0=bfb[:], scalar1=-1.0), [bfb.res], [bfb.res])

        def load(tb):
            i = tb % 2
            dma("sp", xf[i][:], xmT_d[b, :, :, tb * 512:tb * 512 + 520].rearrange("c p t -> p c t"),
                reads=[DR("xmT", b, c) for c in range(8)] + [DR("xmTpad", b)], writes=[xf[i].res])

        load(0)
        cv_i = [0]
        LV = int(os.environ.get("CBIS", "9"))
        for tb in range(8):
            i = tb % 2
            if tb + 1 < 8:
                load(tb + 1)
            for c in range(8 if LV >= 2 else 0):
                bk = ps[cv_i[0] % 2]
                cv_i[0] += 1
                for w in range(4):
                    mm(bk[:, :], cdg[:, c, w, :], xf[i][:, c, 5 + w:5 + w + 512], w == 0, w == 3, [cdg.res, xf[i].res], [bk.res])
                act(xc[i][:, c, :], bk[:, :], AF.Silu, [bk.res, cb.res], [xc[i].res], bias=cb[:, c:c + 1])
                if LV < 3:
                    continue
                for (wt, srcv, sres, dstt, bi_) in ((wq, xc[i][:, c, :], xc[i].res, qs[i], 2),
                                                   (wk, xc[i][:, c, :], xc[i].res, ks[i], 3),
                                                   (wv, xf[i][:, c, 8:520], xf[i].res, vs[i], 2)):
                    bk2 = ps[bi_] if wt is not wv else ps[2 + (c % 2)]
                    mm(bk2[:, :], wt[:, c, :], srcv, True, True, [wt.res, sres], [bk2.res])
                    copy_evac(dstt[:, c, :], bk2[:, :], [bk2.res], [dstt.res])
                if LV < 4:
                    continue
                for (wt, lsrc, off, dstt, bi_) in ((wk, xc[i], 0, ktm[i], 4), (wv, xf[i], 8, vtm[i], 5)):
                    bk3 = ps[bi_]
                    for ti in range(4):
                        mm(bk3[:, ti * 128:(ti + 1) * 128], lsrc[:, c, off + ti * 128:off + (ti + 1) * 128], wt[:, c, :],
                           True, True, [wt.res, lsrc.res], [bk3.res])
                    copy_evac(dstt[:, :, c * 128:(c + 1) * 128], bk3[:, :].rearrange("p (t c) -> p t c", t=4),
                              [bk3.res], [dstt.res])
            bg = ps[6]
            for ti in range(4 if LV >= 5 else 0):
                k_ = 0
                for (srct, w0) in ((qs[i], 0), (ks[i], 8), (vs[i], 16)):
                    for c in range(8):
                        mm(bg[:, ti * 32:(ti + 1) * 32], srct[:, c, ti * 128:(ti + 1) * 128], wif[:, w0 + c, :],
                           k_ == 0, k_ == 23, [srct.res, wif.res], [bg.res])
                        k_ += 1
            dve(lambda e, tb=tb, bg=bg: e.tensor_copy(out=graw[:, tb * 4:(tb + 1) * 4, :],
                                                      in_=bg[:, 0:128].rearrange("p (t g) -> p t g", t=4)[:, :, 0:8]),
                [bg.res], [graw.res])
            dma("sp", qmT_d[b, :, :, tb * 512:(tb + 1) * 512].rearrange("c p t -> p c t"), qs[i][:],
                reads=[qs[i].res], writes=[DR("qmT", b, tb)])
            dma("sp", kmT_d[b, :, :, tb * 512:(tb + 1) * 512].rearrange("c p t -> p c t"), ks[i][:],
                reads=[ks[i].res], writes=[DR("kmT", b, tb)])
            dma("sp", km_d[b, tb * 512:(tb + 1) * 512, :].rearrange("(n p) c -> p n c", p=128), ktm[i][:],
                reads=[ktm[i].res], writes=[DR("km", b, tb)])
            dma("sp", vm_d[b, tb * 512:(tb + 1) * 512, :].rearrange("(n p) c -> p n c", p=128), vtm[i][:],
                reads=[vtm[i].res], writes=[DR("vm", b, tb)])
        if LV < 6:
            return
        li = gt[:, 0, :].rearrange("p (j h) -> p j h", h=4)
        ef = gt[:, 1, :].rearrange("p (j h) -> p j h", h=4)
        for hd in range(4):
            dve(lambda e, hd=hd: e.tensor_scalar_add(out=li[:, :, hd], in0=graw[:, :, hd], scalar1=bib[:, hd:hd + 1]),
                [graw.res, bib.res], [gt.res])
            act(ef[:, :, hd], graw[:, :, 4 + hd], AF.Exp, [graw.res, bfb.res], [gt.res], bias=bfb[:, hd:hd + 1], scale=-1.0)
        if LV < 7:
            return
        act(gt[:, 1, :], gt[:, 1, :], AF.Ln, [gt.res, cst.res], [gt.res], bias=cst[:, 2:3], scale=1.0)
        if LV < 8:
            return
        bc = ps[7]
        dve(lambda e: e.tensor_copy(out=xh[:, 0, :], in_=gt[:, 1, :]), [gt.res], [xh.res])
        dve(lambda e: e.tensor_tensor(out=xr_[:, 0, :], in0=gt[:, 1, :], in1=xh[:, 0, :], op=ALU.subtract),
            [gt.res, xh.res], [xr_.res])
        dve(lambda e: e.tensor_copy(out=xh[:, 1, :], in_=xr_[:, 0, :]), [xr_.res], [xh.res])
        dve(lambda e: e.tensor_tensor(out=xr_[:, 1, :], in0=xr_[:, 0, :], in1=xh[:, 1, :], op=ALU.subtract),
            [xr_.res, xh.res], [xr_.res])
        dve(lambda e: e.tensor_copy(out=xh[:, 2, :], in_=xr_[:, 1, :]), [xr_.res], [xh.res])
        for k_ in range(3):
            mm(bc[:, 0:128], tri_b[:], xh[:, k_, :], k_ == 0, k_ == 2, [tri_b.res, xh.res], [bc.res])
        for k_ in range(3):
            mm(bc[:, 128:256], ones_b[:], xh[:, k_, :], k_ == 0, k_ == 2, [ones_b.res, xh.res], [bc.res])
        if LV == 8:
            return
        act(gp[:, 0, :], bc[:, 0:128], AF.Exp, [bc.res, cst.res], [gp.res], bias=cst[:, 1:2], scale=-1.0)
        if LV == 10:
            return
        dve(lambda e: e.tensor_tensor(out=gt[:, 2, :], in0=bc[:, 0:128], in1=gt[:, 0, :], op=ALU.add),
            [bc.res, gt.res], [gt.res])
        act(gp[:, 1, :], gt[:, 2, :], AF.Exp, [gt.res], [gp.res])
        act(gp[:, 3, :], bc[:, 128:256], AF.Exp, [bc.res], [gp.res], scale=-1.0)
        if LV == 12:
            return
        dve(lambda e: e.tensor_tensor(out=gp[:, 2, :], in0=gp[:, 1, :], in1=gp[:, 3, :], op=ALU.mult),
            [gp.res], [gp.res])
        if LV == 13:
            return
        if dbg:
            dma("sp", gp_dbg[b], gp[:], reads=[gp.res])

    def phase_D(L, b):
        ar = Arena()
        qb_ = [ar.get("qb%d" % i, [128, 8, 512], BF16) for i in range(2)]
        kb_ = [ar.get("kb%d" % i, [128, 8, 512], BF16) for i in range(2)]
        ktm = [ar.get("ktm%d" % i, [128, 4, D], BF16) for i in range(2)]
        vtm = [ar.get("vtm%d" % i, [128, 4, D], BF16) for i in range(2)]
        som = [ar.get("som%d" % i, [128, 4, D], BF16) for i in range(2)]
        szm = [ar.get("szm%d" % i, [128, 4, D], BF16) for i in range(2)]
        C32 = ar.get("C32", [128, 4, 2, 260], F32)
        Cbf = ar.get("Cbf", [128, 4, 2, 260], BF16)
        Cres = [Res("C%d" % i) for i in range(4)]
        Cbres = [Res("Cb%d" % i) for i in range(4)]
        St = [ar.get("St%d" % i, [128, 128], BF16) for i in range(2)]
        vp = [ar.get("vp%d" % i, [128, 4, 260], BF16) for i in range(2)]
        vpp = [ar.get("vpp%d" % i, [128, 4, 260], BF16) for i in range(2)]
        hm = [ar.get("hm%d" % i, [128, D], F32) for i in range(2)]
        sq = ar.get("sq", [128, D], F32)
        gz = ar.get("gz", [128, D], F32)
        mls = [ar.get("mls%d" % i, [128, D], BF16) for i in range(2)]
        hng = ar.get("hng", [128, D], F32)
        mxs = [ar.get("mxs%d" % i, [128, 8, 512], BF16) for i in range(2)]
        sm = [ar.get("sm%d" % i, [128, 8, 4], F32) for i in range(2)]
        ds_ = [ar.get("ds%d" % i, [128, 4], F32) for i in range(4)]
        ps = psum_views()
        psTb = [T(banks[6 + i][:, :].bitcast(BF16), "psTb", True) for i in range(2)]

        dma("sp", hng[:], hng_d[L].partition_broadcast(128), writes=[hng.res])
        dve(lambda e: e.memset(C32[:, :, :, :], 0.0), [], Cres)
        dve(lambda e: e.memset(Cbf[:, :, :, :], 0.0), [], Cbres)

        def load(tb):
            i = tb % 2
            cs = slice(tb * 512, (tb + 1) * 512)
            dma("sp", qb_[i][:], qmT_d[b, :, :, cs].rearrange("c p t -> p c t"), reads=[DR("qmT", b, tb)], writes=[qb_[i].res])
            dma("sp", kb_[i][:], kmT_d[b, :, :, cs].rearrange("c p t -> p c t"), reads=[DR("kmT", b, tb)], writes=[kb_[i].res])
            for (t_, d_, nm) in ((ktm, km_d, "km"), (vtm, vm_d, "vm"), (som, som_d, "som"), (szm, szm_d, "szm")):
                dma("sp", t_[i][:], d_[b, cs, :].rearrange("(n p) c -> p n c", p=128), reads=[DR(nm, b, tb)], writes=[t_[i].res])

        load(0)
        cnt = [0]
        for tb in range(8):
            i = tb % 2
            if tb + 1 < 8:
                load(tb + 1)
            mx = mxs[i]
            for jj in range(4):
                j = tb * 4 + jj
                tk = slice(jj * 128, (jj + 1) * 128)
                vpi, vppi = vp[j % 2], vpp[j % 2]
                for hd in range(4):
                    g_ = j * 4 + hd
                    act(vpi[:, hd, 0:256], vtm[i][:, jj, hd * 256:(hd + 1) * 256], AF.Copy, [vtm[i].res, gp.res], [vpi.res],
                        scale=gp[:, 1, g_:g_ + 1])
                    act(vppi[:, hd, 0:256], vtm[i][:, jj, hd * 256:(hd + 1) * 256], AF.Copy, [vtm[i].res, gp.res], [vppi.res],
                        scale=gp[:, 2, g_:g_ + 1])
                p.add("pool", lambda e, vpi=vpi, j=j: e.tensor_copy(out=vpi[:, :, 256], in_=gp[:, 1, j * 4:(j + 1) * 4]),
                      reads=[gp.res], writes=[vpi.res])
                p.add("pool", lambda e, vppi=vppi, j=j: e.tensor_copy(out=vppi[:, :, 256], in_=gp[:, 2, j * 4:(j + 1) * 4]),
                      reads=[gp.res], writes=[vppi.res])
                hmi = hm[j % 2]
                dve(lambda e, i=i, jj=jj: e.tensor_tensor(out=gz[:], in0=hng[:], in1=szm[i][:, jj, :], op=ALU.mult),
                    [hng.res, szm[i].res], [gz.res])
                def hv_(hd):
                    k_ = j * 4 + hd
                    return k_, ps[k_ % 2], ps[2 + k_ % 2], St[k_ % 2], ds_[k_ % 4]

                def stage1(hd):
                    k_, bs, bn, sti, dsi = hv_(hd)
                    for half in range(2):
                        mm(bs[:, 0:128], kb_[i][:, 2 * hd + half, tk], qb_[i][:, 2 * hd + half, tk], half == 0, half == 1,
                           [kb_[i].res, qb_[i].res], [bs.res])
                    dve(lambda e: e.tensor_tensor(out=sti[:], in0=bs[:, 0:128], in1=tri[:], op=ALU.mult),
                        [bs.res, tri.res], [sti.res])

                def stage2(hd):
                    for half in range(2):
                        bd = ps[4 + half]
                        mm(bd[:, 0:257], ktm[i][:, jj, hd * 256 + half * 128:hd * 256 + (half + 1) * 128], vppi[:, hd, 0:257],
                           True, True, [ktm[i].res, vppi.res], [bd.res])

                def stage3(hd):
                    k_, bs, bn, sti, dsi = hv_(hd)
                    g_ = k_
                    for half in range(2):
                        mm(bn[:, 0:257], qb_[i][:, 2 * hd + half, tk], Cbf[:, hd, half, 0:257], half == 0, False,
                           [qb_[i].res, Cbres[hd]], [bn.res])
                    mm(bn[:, 0:257], sti[:], vpi[:, hd, 0:257], False, True, [sti.res, vpi.res], [bn.res])
                    for half in range(2):
                        bd = ps[4 + half]
                        dve(lambda e, half=half, bd=bd: e.scalar_tensor_tensor(
                            out=C32[:, hd, half, 0:257], in0=C32[:, hd, half, 0:257], scalar=gp[:, 3, g_:g_ + 1],
                            in1=bd[:, 0:257], op0=ALU.mult, op1=ALU.add), [Cres[hd], gp.res, bd.res], [Cres[hd]])
                    act(Cbf[:, hd, :, 0:257], C32[:, hd, :, 0:257], AF.Copy, [Cres[hd]], [Cbres[hd]])
                    act(dsi[:, 0:1], bn[:, 256:257], AF.Abs, [bn.res, gp.res], [dsi.res], scale=gp[:, 0, g_:g_ + 1])
                    dve(lambda e: e.tensor_scalar_max(out=dsi[:, 1:2], in0=dsi[:, 0:1], scalar1=1.0), [dsi.res], [dsi.res])
                    dve(lambda e: e.reciprocal(out=dsi[:, 2:3], in_=dsi[:, 1:2]), [dsi.res], [dsi.res])
                    dve(lambda e: e.tensor_tensor(out=dsi[:, 3:4], in0=dsi[:, 2:3], in1=gp[:, 0, g_:g_ + 1], op=ALU.mult),
                        [dsi.res, gp.res], [dsi.res])
                    dve(lambda e, hmi=hmi, i=i, jj=jj: e.scalar_tensor_tensor(
                        out=hmi[:, hd * 256:(hd + 1) * 256], in0=bn[:, 0:256], scalar=dsi[:, 3:4],
                        in1=som[i][:, jj, hd * 256:(hd + 1) * 256], op0=ALU.mult, op1=ALU.mult),
                        [bn.res, dsi.res, som[i].res], [hmi.res])

                stage1(0)
                stage2(0)
                for hd in range(4):
                    if hd + 1 < 4:
                        stage1(hd + 1)
                    stage3(hd)
                    if hd + 1 < 4:
                        stage2(hd + 1)
                smi = sm[j % 2]
                hv = hmi[:, :].rearrange("p (h d) -> p h d", h=4)
                dve(lambda e, smi=smi, hv=hv: e.reduce_sum(out=smi[:, 0, :], in_=hv, axis=AX.X), [hmi.res], [smi.res])
                act(sq[:], hmi[:], AF.Square, [hmi.res], [sq.res])
                dve(lambda e, smi=smi: e.reduce_sum(out=smi[:, 1, :], in_=sq[:, :].rearrange("p (h d) -> p h d", h=4), axis=AX.X),
                    [sq.res], [smi.res])
                dve(lambda e, smi=smi: e.tensor_scalar_mul(out=smi[:, 2, :], in0=smi[:, 0, :], scalar1=1.0 / 256), [smi.res], [smi.res])
                dve(lambda e, smi=smi: e.tensor_tensor(out=smi[:, 3, :], in0=smi[:, 2, :], in1=smi[:, 2, :], op=ALU.mult), [smi.res], [smi.res])
                dve(lambda e, smi=smi: e.scalar_tensor_tensor(out=smi[:, 4, :], in0=smi[:, 1, :], scalar=1.0 / 256, in1=smi[:, 3, :],
                                                              op0=ALU.mult, op1=ALU.subtract), [smi.res], [smi.res])
                act(smi[:, 5, :], smi[:, 4, :], AF.Sqrt, [smi.res, cst.res], [smi.res], bias=cst[:, 0:1], scale=1.0)
                dve(lambda e, smi=smi: e.reciprocal(out=smi[:, 6, :], in_=smi[:, 5, :]), [smi.res], [smi.res])
                for hd in range(4):
                    dve(lambda e, hd=hd, smi=smi, hmi=hmi: e.tensor_scalar(
                        out=hmi[:, hd * 256:(hd + 1) * 256], in0=hmi[:, hd * 256:(hd + 1) * 256],
                        scalar1=smi[:, 2, hd:hd + 1], scalar2=smi[:, 6, hd:hd + 1], op0=ALU.subtract, op1=ALU.mult),
                        [hmi.res, smi.res], [hmi.res])
                ml = mls[j % 2]
                dve(lambda e, ml=ml, hmi=hmi: e.tensor_tensor(out=ml[:], in0=hmi[:], in1=gz[:], op=ALU.mult),
                    [hmi.res, gz.res], [ml.res])
                pt = psTb[j % 2]
                for c in range(8):
                    p.add("pe", lambda e, pt=pt, ml=ml, c=c: e.transpose(pt[:, c * 128:(c + 1) * 128], ml[:, c * 128:(c + 1) * 128], ident[:]),
                          reads=[ml.res, ident.res], writes=[pt.res])
                act(mx[:, :, tk], pt[:, :].rearrange("p (c t) -> p c t", c=8), AF.Copy, [pt.res], [mx.res])
            dma("sp", mixT_d[b, 8:16, :, tb * 512:(tb + 1) * 512].rearrange("c p t -> p c t"), mx[:],
                reads=[mx.res], writes=[DR("mixT", b, 8 + tb)])

    def phase_E(L, b):
        ar = Arena()
        wo = ar.get("wo", [128, 16, D], BF16)
        mxl = [ar.get("mxl%d" % i, [128, 16, 512], BF16) for i in range(2)]
        xr = [ar.get("xr%d" % i, [128, 4, D], F32) for i in range(2)]
        xo = [ar.get("xo%d" % i, [128, 4, D], F32) for i in range(2)]
        gf = ar.get("gf", [128, D], F32)
        junk = ar.get("junkE", [128, D], F32)
        stt = ar.get("sttE", [128, 3, 32], F32)
        ps = psum_views()
        last = (L == DEPTH - 1)
        src = x_d if L == 0 else x1_d
        dma("pool", wo[:], wout_d[L].rearrange("(k p) c -> p k c", p=128), writes=[wo.res])
        if last:
            dma("sp", gf[:], fing_d.partition_broadcast(128), writes=[gf.res])

        def load(tb):
            i = tb % 2
            cs = slice(tb * 512, (tb + 1) * 512)
            dma("sp", mxl[i][:], mixT_d[b, :, :, cs].rearrange("c p t -> p c t"),
                reads=[DR("mixT", b, k_) for k_ in range(16)], writes=[mxl[i].res])
            dma("sp", xr[i][:], src[b, cs, :].rearrange("(n p) c -> p n c", p=128),
                reads=[DR("x1", b, tb)] if L > 0 else [], writes=[xr[i].res])

        load(0)
        k_ = 0
        for tb in range(8):
            i = tb % 2
            if tb + 1 < 8:
                load(tb + 1)
            for n in range(4):
                for half in range(2):
                    bk = ps[k_ % 4]
                    k_ += 1
                    for kc in range(16):
                        mm(bk[:, :], mxl[i][:, kc, n * 128:(n + 1) * 128], wo[:, kc, half * 512:(half + 1) * 512],
                           kc == 0, kc == 15, [mxl[i].res, wo.res], [bk.res])
                    dve(lambda e, i=i, n=n, half=half, bk=bk: e.tensor_tensor(
                        out=xo[i][:, n, half * 512:(half + 1) * 512], in0=bk[:, :],
                        in1=xr[i][:, n, half * 512:(half + 1) * 512], op=ALU.add), [bk.res, xr[i].res], [xo[i].res])
                if last:
                    ti = tb * 4 + n
                    act(junk[:], xo[i][:, n, :], AF.Square, [xo[i].res], [junk.res, stt.res], accum=stt[:, 0, ti:ti + 1])
                    act(stt[:, 1, ti:ti + 1], stt[:, 0, ti:ti + 1], AF.Sqrt, [stt.res, cst.res], [stt.res],
                        bias=cst[:, 0:1], scale=1.0 / D)
                    dve(lambda e, ti=ti: e.reciprocal(out=stt[:, 2, ti:ti + 1], in_=stt[:, 1, ti:ti + 1]), [stt.res], [stt.res])
                    dve(lambda e, i=i, n=n, ti=ti: e.scalar_tensor_tensor(
                        out=xo[i][:, n, :], in0=xo[i][:, n, :], scalar=stt[:, 2, ti:ti + 1], in1=gf[:],
                        op0=ALU.mult, op1=ALU.mult), [xo[i].res, stt.res, gf.res], [xo[i].res])
            dst = y_d if last else x1_d
            dma("sp", dst[b, tb * 512:(tb + 1) * 512, :].rearrange("(n p) c -> p n c", p=128), xo[i][:],
                reads=[xo[i].res], writes=[] if last else [DR("x1", b, tb)])

    fns = {"A": phase_A, "B": phase_B, "C": phase_C, "D": phase_D, "E": phase_E}
    for L in layers:
        for b in seqs:
            for ph in phases:
                fns[ph](L, b)
                p.barrier()
    p.emit()
    return nc


def host_consts():
    ident = np.eye(128, dtype=np.float32).astype(ml_dtypes.bfloat16)
    tri = np.triu(np.ones((128, 128), np.float32))
    slopes = np.exp2(-8.0 * np.arange(1, 9, dtype=np.float32) / 8.0)
    i = np.arange(128)[:, None]
    j = np.arange(128)[None, :]
    am = np.zeros((128, 48, 128), np.float32)
    for h in range(8):
        for di, d_ in enumerate(DILS):
            cur = np.where(j >= i, -slopes[h] * d_ * (j - i), NEG)
            prev = np.where(j <= i, -slopes[h] * d_ * (128 + j - i), NEG)
            am[:, (h * 3 + di) * 2, :] = cur
            am[:, (h * 3 + di) * 2 + 1, :] = prev
    return ident, tri, am.astype(ml_dtypes.bfloat16)


def host_layout(inputs):
    f = lambda a: np.ascontiguousarray(np.asarray(a, dtype=np.float32))
    conv_w = f(inputs["conv_w"])
    cdiag = np.zeros((DEPTH, 128, 8, 4, 128), np.float32)
    pidx = np.arange(128)
    for L in range(DEPTH):
        for c in range(8):
            for w in range(4):
                cdiag[L, pidx, c, w, pidx] = conv_w[L, w, c * 128 + pidx]
    cb = np.ascontiguousarray(f(inputs["conv_b"]).reshape(DEPTH, 8, 128).transpose(0, 2, 1))

    def bd(w):
        w = f(w)
        out = np.zeros((DEPTH, 128, 8, 128), np.float32)
        for L in range(DEPTH):
            for c in range(8):
                for n in range(32):
                    out[L, n * 4:(n + 1) * 4, c, n * 4:(n + 1) * 4] = w[L, c * 32 + n]
        return out

    ident, tri, am = host_consts()
    common = {
        "norm_g": f(inputs["norm_g"]), "w_in": f(inputs["w_in"]), "cdiag": cdiag, "cb": cb,
        "wq_bd": bd(inputs["w_qm"]), "wk_bd": bd(inputs["w_km"]), "wv_bd": bd(inputs["w_vm"]),
        "w_if": np.ascontiguousarray(np.pad(f(inputs["w_if"]).reshape(DEPTH, 24, 128, 8).transpose(0, 2, 1, 3), ((0, 0), (0, 0), (0, 0), (0, 24)))), "b_i": np.ascontiguousarray(np.broadcast_to(f(inputs["b_i"])[:, None, :], (DEPTH, 128, 4))), "b_f": np.ascontiguousarray(np.broadcast_to(f(inputs["b_f"])[:, None, :], (DEPTH, 128, 4))),
        "hn_g": f(inputs["hn_g"]), "w_out": f(inputs["w_out"]), "final_g": f(inputs["final_g"]),
        "ident": ident, "tri": tri, "amask": am,
    }
    return common


def kernel(**inputs):
    x = np.ascontiguousarray(np.asarray(inputs["x"], dtype=np.float32))
    common = host_layout(inputs)
    nc = build()
    in_maps = []
    for c in range(8):
        m = dict(common)
        m["x"] = x[c * NSEQ:(c + 1) * NSEQ]
        in_maps.append(m)
    res = run_bass_kernel_spmd(nc, in_maps, core_ids=list(range(8)))
    return np.concatenate([r["y"] for r in res.results], axis=0).astype(np.float32)
```
